# Optimizing a Trainium2 kernel written in Bass

```python
import math
import jax, jax.numpy as jnp
from jax import lax
import numpy as np

D_MODEL = 1024
BATCH = 32
SEQ = 2048
DEPTH = 2

CTX_LEN = 256
GRID_W = 64
EPS = 1e-6

N_HEADS = 8
HEAD_DIM = 64
V_DIM = 2 * HEAD_DIM
Q_WIDTH = N_HEADS * 2 * HEAD_DIM
K_WIDTH = N_HEADS * 2 * HEAD_DIM
V_WIDTH = N_HEADS * V_DIM
ATTN_WIDTH = V_WIDTH
Q_BLOCK = 128
ROPE_THETA = 10000.0
AXIS_DIM = HEAD_DIM // 2
ROPE_PAIRS = AXIS_DIM // 2

POOL_WINDOWS = (2, 4, 8, 16)
POOL_GROUPS = len(POOL_WINDOWS)
POOL_WIDTH = 512
POOL_GROUP_DIM = POOL_WIDTH // POOL_GROUPS

N_BRANCHES = 2

OFF_POOL = 0
OFF_Q = OFF_POOL + POOL_WIDTH
OFF_K = OFF_Q + Q_WIDTH
OFF_V = OFF_K + K_WIDTH
OFF_G = OFF_V + V_WIDTH
IN_WIDTH = OFF_G + N_BRANCHES * D_MODEL

D_FF = 2752
N_EXPERTS = 8
TOP_K = 2
D_FF_EXPERT = 1408
N_DENSE = (DEPTH + 1) // 2
N_MOE = DEPTH // 2

kernel_name = "hybrid_pool_diffattn_moe_dit"


def rms_norm(x, g):
    xf = x.astype(jnp.float32)
    y = xf * lax.rsqrt(jnp.mean(xf * xf, axis=-1, keepdims=True) + EPS)
    return (y * g.astype(jnp.float32)).astype(x.dtype)


def modulate(h, shift, scale):
    return h * (1 + scale) + shift


def lambda_init(layer):
    return 0.8 - 0.6 * math.exp(-0.3 * layer)


def axial_rope_tables(L, dtype):
    rows = L // GRID_W
    row = jnp.repeat(jnp.arange(rows, dtype=jnp.float32), GRID_W)
    col = jnp.tile(jnp.arange(GRID_W, dtype=jnp.float32), rows)
    inv = ROPE_THETA ** (-jnp.arange(ROPE_PAIRS, dtype=jnp.float32) * 2.0 / AXIS_DIM)
    ang = jnp.stack([row[:, None] * inv, col[:, None] * inv], axis=1)
    return jnp.cos(ang).astype(dtype), jnp.sin(ang).astype(dtype)


def apply_axial_rope(u, cos, sin):
    shp = u.shape
    ur = u.reshape(shp[:-1] + (2, 2, ROPE_PAIRS))
    a, b = ur[..., 0, :], ur[..., 1, :]
    cs, sn = cos[:, None, None], sin[:, None, None]
    out = jnp.stack([a * cs - b * sn, a * sn + b * cs], axis=-2)
    return out.reshape(shp)


def centred_pool_minus_self(u):
    L = u.shape[1]
    uf = u.astype(jnp.float32)
    cs = jnp.concatenate([jnp.zeros_like(uf[:, :1]), jnp.cumsum(uf, axis=1)], axis=1)
    t = np.arange(L)
    means = []
    for g, w in enumerate(POOL_WINDOWS):
        lo = np.clip(t - w // 2, 0, L)
        hi = np.clip(t + w // 2, 0, L)
        cnt = (hi - lo).astype(np.float32)[None, :, None]
        csg = cs[..., g * POOL_GROUP_DIM:(g + 1) * POOL_GROUP_DIM]
        means.append((csg[:, hi] - csg[:, lo]) / cnt)
    return (jnp.concatenate(means, axis=-1) - uf).astype(u.dtype)


def pool_branch(u, pool_w_l, pool_scale_l):
    B, L, _ = u.shape
    m = centred_pool_minus_self(u).reshape(B, L, POOL_GROUPS, POOL_GROUP_DIM)
    y = jnp.einsum('blgc,gcd->blgd', m, pool_w_l).reshape(B, L, POOL_WIDTH)
    return y * pool_scale_l


def split_heads_q(zq):
    B, L, _ = zq.shape
    return zq.reshape(B, L, N_HEADS, 2, HEAD_DIM)


def split_heads_v(zv):
    B, L, _ = zv.shape
    return zv.reshape(B, L, N_HEADS, V_DIM)


def diff_attend(q, k, v, lam):
    s = jnp.einsum('bqhmd,bkhmd->bhmqk', q, k).astype(jnp.float32)
    p = jax.nn.softmax(s, axis=-1)
    a = p[:, :, 0] - lam * p[:, :, 1]
    return jnp.einsum('bhqk,bkhe->bqhe', a.astype(v.dtype), v)


def latent_attention(q, k_all, v_all, lam):
    B, L = q.shape[:2]
    nb = L // Q_BLOCK
    qb = q.reshape(B, nb, Q_BLOCK, N_HEADS, 2, HEAD_DIM).swapaxes(0, 1)
    ob = lax.map(lambda qq: diff_attend(qq, k_all, v_all, lam), qb)
    return ob.swapaxes(0, 1).reshape(B, L, N_HEADS, V_DIM)


def head_norm(o, g, lam0):
    B, L = o.shape[:2]
    return (rms_norm(o, g) * (1.0 - lam0)).reshape(B, L, ATTN_WIDTH)


def merge_branches(z, pool_y, attn_y, w_pool_proj_l, w_attn_proj_l, w_out_l):
    g = jax.nn.sigmoid(z[..., OFF_G:].astype(jnp.float32)).astype(z.dtype)
    g_pool, g_attn = g[..., :D_MODEL], g[..., D_MODEL:]
    y = g_pool * (pool_y @ w_pool_proj_l) + g_attn * (attn_y @ w_attn_proj_l)
    return y @ w_out_l


def swiglu(t, w1, w3, w2):
    return (jax.nn.silu(t @ w1) * (t @ w3)) @ w2


def moe_swiglu(h, w_router, we1, we3, we2):
    B, L, D = h.shape
    t = h.reshape(B * L, D)
    logits = (t @ w_router).astype(jnp.float32)
    top_v, top_i = lax.top_k(logits, TOP_K)
    wts = jax.nn.softmax(top_v, axis=-1)
    gate = jnp.sum(jax.nn.one_hot(top_i, N_EXPERTS, dtype=jnp.float32) * wts[..., None], axis=1)
    out = jnp.zeros((B * L, D), jnp.float32)
    for e in range(N_EXPERTS):
        out = out + gate[:, e:e + 1] * swiglu(t, we1[e], we3[e], we2[e]).astype(jnp.float32)
    return out.astype(h.dtype).reshape(B, L, D)


def channel_mixer(l, h, ffn_w1, ffn_w3, ffn_w2, router_w, moe_w1, moe_w3, moe_w2):
    if l % 2 == 0:
        i = l // 2
        B, L, D = h.shape
        return swiglu(h.reshape(B * L, D), ffn_w1[i], ffn_w3[i], ffn_w2[i]).reshape(B, L, D)
    i = l // 2
    return moe_swiglu(h, router_w[i], moe_w1[i], moe_w3[i], moe_w2[i])


def setup_inputs(seed: int = 0) -> dict:
    key = jax.random.key(seed)
    ks = iter(jax.random.split(key, 32))

    def nrm(shape, scale):
        return jax.random.normal(next(ks), shape, jnp.float32) * scale

    D = D_MODEL
    return {
        "x": nrm((BATCH, SEQ, D), 1.0),
        "c": nrm((BATCH, D), 1.0),
        "ctx": nrm((BATCH, CTX_LEN, D), 1.0),
        "c_ctx": nrm((D,), 1.0),
        "w_mod": nrm((DEPTH, D, 6 * D), 0.5 * D ** -0.5),
        "b_mod": nrm((DEPTH, 6 * D), 0.02),
        "norm1_g": 1.0 + nrm((DEPTH, D), 0.02),
        "norm2_g": 1.0 + nrm((DEPTH, D), 0.02),
        "w_in": nrm((DEPTH, D, IN_WIDTH), D ** -0.5),
        "pool_w": nrm((DEPTH, POOL_GROUPS, POOL_GROUP_DIM, POOL_GROUP_DIM), POOL_GROUP_DIM ** -0.5),
        "pool_scale": 1.0 + nrm((DEPTH, POOL_WIDTH), 0.1),
        "lam_q1": nrm((DEPTH, HEAD_DIM), 0.1),
        "lam_k1": nrm((DEPTH, HEAD_DIM), 0.1),
        "lam_q2": nrm((DEPTH, HEAD_DIM), 0.1),
        "lam_k2": nrm((DEPTH, HEAD_DIM), 0.1),
        "subln_g": 1.0 + nrm((DEPTH, V_DIM), 0.02),
        "w_pool_proj": nrm((DEPTH, POOL_WIDTH, D), POOL_WIDTH ** -0.5),
        "w_attn_proj": nrm((DEPTH, ATTN_WIDTH, D), ATTN_WIDTH ** -0.5),
        "w_out": nrm((DEPTH, D, D), D ** -0.5),
        "ffn_w1": nrm((N_DENSE, D, D_FF), D ** -0.5),
        "ffn_w3": nrm((N_DENSE, D, D_FF), D ** -0.5),
        "ffn_w2": nrm((N_DENSE, D_FF, D), D_FF ** -0.5),
        "router_w": nrm((N_MOE, D, N_EXPERTS), D ** -0.5),
        "moe_w1": nrm((N_MOE, N_EXPERTS, D, D_FF_EXPERT), D ** -0.5),
        "moe_w3": nrm((N_MOE, N_EXPERTS, D, D_FF_EXPERT), D ** -0.5),
        "moe_w2": nrm((N_MOE, N_EXPERTS, D_FF_EXPERT, D), D_FF_EXPERT ** -0.5),
        "final_g": 1.0 + nrm((D,), 0.02),
    }


def reference(x, c, ctx, c_ctx, w_mod, b_mod, norm1_g, norm2_g, w_in, pool_w, pool_scale,
              lam_q1, lam_k1, lam_q2, lam_k2, subln_g, w_pool_proj, w_attn_proj, w_out,
              ffn_w1, ffn_w3, ffn_w2, router_w, moe_w1, moe_w3, moe_w2, final_g):
    L = x.shape[1]
    cos, sin = axial_rope_tables(L, x.dtype)
    q_scale = HEAD_DIM ** -0.5
    xc = ctx
    s_lat = jax.nn.silu(c)
    s_ctx = jax.nn.silu(c_ctx)
    for l in range(DEPTH):
        last = l == DEPTH - 1
        lam0 = lambda_init(l)
        lam = (jnp.exp(jnp.sum(lam_q1[l].astype(jnp.float32) * lam_k1[l].astype(jnp.float32)))
               - jnp.exp(jnp.sum(lam_q2[l].astype(jnp.float32) * lam_k2[l].astype(jnp.float32)))
               + lam0)
        mod = jnp.split((s_lat @ w_mod[l] + b_mod[l])[:, None, :], 6, axis=-1)
        modc = jnp.split(s_ctx @ w_mod[l] + b_mod[l], 6, axis=-1)

        h = modulate(rms_norm(x, norm1_g[l]), mod[0], mod[1])
        hc = modulate(rms_norm(xc, norm1_g[l]), modc[0], modc[1])
        z = h @ w_in[l]
        if last:
            kvc = hc @ w_in[l][:, OFF_K:OFF_G]
            kc, vc = kvc[..., :K_WIDTH], kvc[..., K_WIDTH:]
        else:
            zc = hc @ w_in[l]
            kc, vc = zc[..., OFF_K:OFF_V], zc[..., OFF_V:OFF_G]
        kc_h = split_heads_q(kc)
        vc_h = split_heads_v(vc)

        q = apply_axial_rope(split_heads_q(z[..., OFF_Q:OFF_K]), cos, sin) * q_scale
        k = apply_axial_rope(split_heads_q(z[..., OFF_K:OFF_V]), cos, sin)
        v = split_heads_v(z[..., OFF_V:OFF_G])
        k_all = jnp.concatenate([kc_h, k], axis=1)
        v_all = jnp.concatenate([vc_h, v], axis=1)
        attn_y = head_norm(latent_attention(q, k_all, v_all, lam), subln_g[l], lam0)
        pool_y = pool_branch(z[..., OFF_POOL:OFF_Q], pool_w[l], pool_scale[l])
        x = x + mod[2] * merge_branches(z, pool_y, attn_y, w_pool_proj[l], w_attn_proj[l], w_out[l])

        if not last:
            qc = split_heads_q(zc[..., OFF_Q:OFF_K]) * q_scale
            attn_yc = head_norm(diff_attend(qc, kc_h, vc_h, lam), subln_g[l], lam0)
            pool_yc = pool_branch(zc[..., OFF_POOL:OFF_Q], pool_w[l], pool_scale[l])
            xc = xc + modc[2] * merge_branches(zc, pool_yc, attn_yc, w_pool_proj[l], w_attn_proj[l], w_out[l])

        h2 = modulate(rms_norm(x, norm2_g[l]), mod[3], mod[4])
        x = x + mod[5] * channel_mixer(l, h2, ffn_w1, ffn_w3, ffn_w2, router_w, moe_w1, moe_w3, moe_w2)
        if not last:
            h2c = modulate(rms_norm(xc, norm2_g[l]), modc[3], modc[4])
            xc = xc + modc[5] * channel_mixer(l, h2c, ffn_w1, ffn_w3, ffn_w2, router_w, moe_w1, moe_w3, moe_w2)
    return rms_norm(x, final_g)
```

```python
import math
from contextlib import ExitStack
import numpy as np
import concourse.bass as bass
import concourse.mybir as mybir
from concourse.bass_utils import run_bass_kernel_spmd

F32 = mybir.dt.float32
BF16 = mybir.dt.bfloat16
AF = mybir.ActivationFunctionType
ALU = mybir.AluOpType

ENGS = ("pe", "act", "dve", "pool", "sp")
EPOCH = 30000


class Res:
    __slots__ = ("name", "w", "r")

    def __init__(self, name):
        self.name = name
        self.w = None
        self.r = {}


class Op:
    __slots__ = ("eng", "fn", "deps", "pos", "flag", "dsem", "dval", "waits", "ordinal")

    def __init__(self, eng, fn):
        self.eng = eng
        self.fn = fn
        self.deps = []
        self.pos = -1
        self.flag = False
        self.dsem = None
        self.dval = 0
        self.waits = []
        self.ordinal = 0


class Prog:
    def __init__(self, nc):
        self.nc = nc
        self.ops = {e: [] for e in ENGS}
        self.dma_counts = {}
        self.last_dma = {}
        self.nres = 0

    def res(self, name=None):
        self.nres += 1
        return Res(name or f"r{self.nres}")

    def _add(self, op, reads, writes):
        deps = {}
        for r in reads:
            if r.w is not None:
                deps[id(r.w)] = (r.w, True)
        for w in writes:
            if w.w is not None and id(w.w) not in deps:
                deps[id(w.w)] = (w.w, False)
            for o in w.r.values():
                if id(o) not in deps:
                    deps[id(o)] = (o, False)
        for (o, raw) in deps.values():
            if o is op:
                continue
            if o.dsem is None and op.dsem is None and o.eng == op.eng:
                if (not raw) or op.eng == "pe":
                    continue
            op.deps.append(o)
        op.pos = len(self.ops[op.eng])
        self.ops[op.eng].append(op)
        for r in reads:
            r.r[op.eng if op.dsem is None else ("dma", id(op))] = op
        for w in writes:
            w.w = op
            w.r = {}
        return op

    def op(self, eng, fn, reads=(), writes=()):
        return self._add(Op(eng, fn), reads, writes)

    def dma(self, queue, sem, out, in_, reads=(), writes=()):
        def fn(e, out=out, in_=in_):
            return e.dma_start(out=out, in_=in_)
        op = Op(queue, fn)
        op.dsem = sem
        k = self.dma_counts.get(id(sem), 0) + 1
        self.dma_counts[id(sem)] = k
        op.dval = 16 * k
        assert op.dval < 65000
        prev = self.last_dma.get(id(sem))
        self._add(op, reads, writes)
        if prev is not None and all(d is not prev for d in op.deps):
            op.deps.append(prev)
        self.last_dma[id(sem)] = op
        return op

    def finalize(self):
        for e in ENGS:
            waited_pos = {}
            waited_dma = {}
            for op in self.ops[e]:
                for d in op.deps:
                    if d.dsem is not None:
                        if waited_dma.get(id(d.dsem), 0) >= d.dval:
                            continue
                        waited_dma[id(d.dsem)] = d.dval
                        op.waits.append(("dma", d))
                    else:
                        if waited_pos.get(d.eng, -1) >= d.pos:
                            continue
                        waited_pos[d.eng] = d.pos
                        d.flag = True
                        op.waits.append(("eng", d))
        self.nflag = {}
        for e in ENGS:
            n = 0
            for op in self.ops[e]:
                if op.dsem is None and op.flag:
                    op.ordinal = n
                    n += 1
            self.nflag[e] = n

    def emit(self, mksem, final_waits=()):
        nc = self.nc
        self.finalize()
        esems = {}
        for e in ENGS:
            esems[e] = [mksem(f"es_{e}_{i}") for i in range(self.nflag[e] // EPOCH + 1)]
        engobj = {"pe": "tensor", "act": "scalar", "dve": "vector", "pool": "gpsimd", "sp": "sync"}

        def run(e, eng):
            for op in self.ops[e]:
                for kind, d in op.waits:
                    if kind == "dma":
                        eng.wait_ge(d.dsem, d.dval)
                    else:
                        eng.wait_ge(esems[d.eng][d.ordinal // EPOCH], d.ordinal % EPOCH + 1)
                ins = op.fn(eng)
                if op.dsem is not None:
                    ins.then_inc(op.dsem, 16)
                elif op.flag:
                    ins.then_inc(esems[e][op.ordinal // EPOCH], 1)
            if e == "sp":
                for (s, v) in final_waits:
                    eng.wait_ge(s, v)

        with nc.Block() as block:
            for e in ENGS:
                getattr(block, engobj[e])(lambda eng, e=e: run(e, eng))

    def stats(self):
        return {e: len(self.ops[e]) for e in ENGS}


D = 1024
KC = 8
NH = 8
GRID_W = 64
EPS = 1e-6
POOLW = (2, 4, 8, 16)
OFF_POOL, OFF_Q, OFF_K, OFF_V, OFF_G = 0, 512, 1536, 2560, 3584
IN_W = 5632
DFF = 2752
NE = 8
DFE = 1408
VL = 73
NV = 2 * VL + 8
WSLOT = 4096
HAL = 8


def lambda_init(layer):
    return 0.8 - 0.6 * math.exp(-0.3 * layer)


class Cfg:
    def __init__(self, NB=4, L=2048, C=256, depth=2, NW=3):
        self.NB, self.L, self.C, self.depth, self.NW = NB, L, C, depth, NW
        self.TQ = 512
        self.NT = L // 512
        self.S = C + L
        self.NKC = self.S // 128


def build(cfg):
    NB, L, C, NT, S, NKC = cfg.NB, cfg.L, cfg.C, cfg.NT, cfg.S, cfg.NKC
    depth = cfg.depth
    nc = bass.Bass("TRN2", target_bir_lowering=False)

    def din(name, shape):
        return nc.dram_tensor(name, list(shape), F32, kind="ExternalInput")

    x_d = din("x", [NB, L, D])
    ctx_d = din("ctx", [NB, C, D])
    cT_d = din("cT", [128, KC, NB + 1])
    vecs_d = din("vecs", [128, NV])
    ident_d = din("ident", [128, 128])
    rmat_d = din("rmat", [128, 128])
    cos_d = din("cosT", [128, L])
    sin_d = din("sinT", [128, L])
    edge_d = din("edge", [128, 4, 16])
    sel_d = din("sel", [8, 8 * 128])
    w_mod_d = din("w_mod", [2, D, 6 * D])
    w_in_d = din("w_in", [2, D, IN_W])
    pool_w_d = din("pool_w", [2, 4, 128, 128])
    w_pp_d = din("w_pool_proj", [2, 512, D])
    w_ap_d = din("w_attn_proj", [2, D, D])
    w_out_d = din("w_out", [2, D, D])
    ffn_w1_d = din("ffn_w1", [1, D, DFF])
    ffn_w3_d = din("ffn_w3", [1, D, DFF])
    ffn_w2_d = din("ffn_w2", [1, DFF, D])
    router_d = din("router_w", [1, D, NE])
    moe_w1_d = din("moe_w1", [1, NE, D, DFE])
    moe_w3_d = din("moe_w3", [1, NE, D, DFE])
    moe_w2_d = din("moe_w2", [1, NE, DFE, D])
    out_d = nc.dram_tensor("out", [NB, L, D], F32, kind="ExternalOutput")
    xs_d = nc.dram_tensor("xs", [2, NB, NT + 1, 128, KC * 512], F32)

    es = ExitStack()
    with es:
        def sb(name, shape, dt):
            return es.enter_context(nc.sbuf_tensor("sb_" + name, list(shape), dt))

        def mksem(name):
            return es.enter_context(nc.semaphore(name))

        P = Prog(nc)
        R = P.res

        KT = sb("KT", [128, NH, S], BF16)
        Vt = sb("Vt", [128, NKC, D], BF16)
        cosT = sb("cosT", [128, L], BF16)
        sinT = sb("sinT", [128, L], BF16)
        wsl = [sb(f"wsl{i}", [128, WSLOT], BF16) for i in range(cfg.NW)]
        xT = sb("xT", [128, KC, 512], F32)
        xh = sb("xh", [128, KC, 16], F32)
        hT = sb("hT", [128, KC, 512], BF16)
        hh = sb("hh", [128, KC, 16], BF16)
        NTMP = 6
        tmp = [sb(f"tmp{i}", [128, 528], F32) for i in range(NTMP)]
        QT = sb("QT", [128, NH, 512], BF16)
        PT = [sb(f"PT{i}", [128, 512], BF16) for i in range(4)]
        AY = sb("AY", [128, NH, 512], BF16)
        upad = sb("upad", [128, 4, 528], F32)
        PY = sb("PY", [128, 4, 512], BF16)
        hidraw = sb("hidraw", [128, 2816], F32)
        hid = hidraw.bitcast(BF16)[:, 0:5632].rearrange("p (k t) -> p k t", k=11)
        gbf = [sb(f"gbf{i}", [128, 512], BF16) for i in range(4)]
        xtm = [hidraw[:, i * 1024:(i + 1) * 1024] for i in range(2)]
        ident = sb("ident", [128, 128], F32)
        rmat = sb("rmat", [128, 128], BF16)
        ones_bf = sb("ones_bf", [128, 128], BF16)
        ones_f = sb("ones_f", [128, 128], F32)
        vecs = sb("vecs", [128, NV], F32)
        edge = sb("edge", [128, 4, 16], F32)
        gsel = [sb(f"gsel{i}", [8, 512], BF16) for i in range(2)]
        R_gsel = None
        sT = sb("sT", [128, KC, NB + 1], F32)
        modT = sb("modT", [128, 2, 48, NB + 1], F32)
        A1 = sb("A1", [128, 2, KC, NB + 1], F32)
        A2 = sb("A2", [128, 2, KC, NB + 1], F32)
        lamv = sb("lamv", [128, 2, 4], F32)
        wr = sb("wr", [128, KC, NE], F32)
        wrh = sb("wrh", [128, KC, NE], BF16)
        wrl = sb("wrl", [128, KC, NE], BF16)
        lg = sb("lg", [128, 4, NE], F32)
        gt = sb("gt", [128, 4, NE], F32)
        gtb = sb("gtb", [128, 4, NE], BF16)
        gT8 = sb("gT8", [8, 512], BF16)
        mx8 = sb("mx8", [128, 4, 8], F32)
        sm = sb("sm", [128, 16], F32)

        pb = [es.enter_context(nc.psum_tensor(f"pb{i}", [128, 512], F32)) for i in range(8)]
        Rpb = [R(f"pb{i}") for i in range(8)]
        bank_ctr = [0]

        def bank(lo=0, hi=8):
            i = lo + bank_ctr[0] % (hi - lo)
            bank_ctr[0] += 1
            return i

        R_KT = {}
        R_V = {}
        Rw = [R(f"w{i}") for i in range(cfg.NW)]
        wsem = [mksem(f"wsem{i}") for i in range(cfg.NW)]
        R_xT, R_xh, R_hT, R_hh = R("xT"), R("xh"), R("hT"), R("hh")
        R_xk = [R(f"xTk{k}") for k in range(KC)]
        R_hk = [R(f"hTk{k}") for k in range(KC)]
        Rtmp = [R(f"tmp{i}") for i in range(NTMP)]
        R_QT = [R(f"QT{h}") for h in range(NH)]
        R_PT = [R(f"PT{i}") for i in range(4)]
        R_AY = [R(f"AY{h}") for h in range(NH)]
        R_up = [R(f"up{g}") for g in range(4)]
        R_PY = [R(f"PY{g}") for g in range(4)]
        R_hid = [R(f"hid{i}") for i in range(11)]
        R_gbf = [R(f"gbf{i}") for i in range(4)]
        R_const = R("const")
        R_mod = R("mod")
        R_misc = R("misc")
        R_gate = R("gate")
        R_hf = R("hf")
        R_lg = R("lg")
        R_xs = {}
        tmp_ctr = [0]

        def T():
            i = tmp_ctr[0] % NTMP
            tmp_ctr[0] += 1
            return tmp[i], Rtmp[i]

        sem_c = mksem("sem_c")
        sem_x = [mksem(f"sem_x{i}") for i in range(2)]
        sem_xT = mksem("sem_xT")
        sem_xh = mksem("sem_xh")
        sem_o = [mksem(f"sem_o{i}") for i in range(2)]
        sem_sp = mksem("sem_sp")
        final_waits = []
        dbg_sem = mksem("dbg_sem")
        dbg_names = []

        def dump(name, ap, shape, dt, reads):
            if not getattr(cfg, "debug", False):
                return
            if cfg.debug is not True and not any(name.startswith(p) for p in cfg.debug):
                return
            t = nc.dram_tensor("dbg_" + name, list(shape), dt, kind="ExternalOutput")
            o = P.dma("sp", dbg_sem, t.ap(), ap, reads=reads)
            final_waits.append((dbg_sem, o.dval))
            dbg_names.append("dbg_" + name)
        cfg.dbg_names = dbg_names

        w_ctr = [0]

        wcache = {}
        ssem = [mksem(f"ssem{i}") for i in range(cfg.NW)]

        def wload(src_ap, nk, ncols, key=None):
            s = w_ctr[0] % cfg.NW
            w_ctr[0] += 1
            flat = wsl[s][:, 0:nk * ncols]
            view = flat.rearrange("p (k c) -> p k c", k=nk)
            if key is not None and key in wcache:
                scr, rscr = wcache[key]
                P.dma("sp", wsem[s], flat, scr.ap(), reads=[rscr], writes=[Rw[s]])
                return view, Rw[s]
            P.dma("pool", wsem[s], view, src_ap, writes=[Rw[s]])
            if key is not None and getattr(cfg, "wcache", True):
                scr = nc.dram_tensor(f"ws{len(wcache)}", [128, nk * ncols], BF16)
                rscr = R(f"ws{len(wcache)}")
                P.dma("sp", ssem[s], scr.ap(), flat, reads=[Rw[s]], writes=[rscr])
                wcache[key] = (scr, rscr)
            return view, Rw[s]

        def mm(out, lhsT, rhs, start, stop, reads, writes):
            P.op("pe", lambda e: e.matmul(out, lhsT=lhsT, rhs=rhs, start=start, stop=stop),
                 reads=reads, writes=writes)

        def act(out, in_, func, reads, writes, bias=None, scale=None):
            kw = {}
            if bias is not None:
                kw["bias"] = bias
            if scale is not None:
                kw["scale"] = scale
            P.op("act", lambda e: e.activation(out=out, in_=in_, func=func, **kw), reads=reads, writes=writes)

        def tt(eng, out, in0, in1, op, reads, writes):
            P.op(eng, lambda e: e.tensor_tensor(out=out, in0=in0, in1=in1, op=op), reads=reads, writes=writes)

        def stt(eng, out, in0, scalar, in1, op0, op1, reads, writes):
            P.op(eng, lambda e: e.scalar_tensor_tensor(out=out, in0=in0, scalar=scalar, in1=in1, op0=op0, op1=op1),
                 reads=reads, writes=writes)

        def ts(eng, out, in0, s1, s2, op0, op1, reads, writes):
            if s2 is None:
                P.op(eng, lambda e: e.tensor_scalar(out=out, in0=in0, scalar1=s1, scalar2=None, op0=op0),
                     reads=reads, writes=writes)
            else:
                P.op(eng, lambda e: e.tensor_scalar(out=out, in0=in0, scalar1=s1, scalar2=s2, op0=op0, op1=op1),
                     reads=reads, writes=writes)

        def cp(eng, out, in_, reads, writes):
            if eng == "act":
                act(out, in_, AF.Copy, reads, writes)
            else:
                P.op(eng, lambda e: e.tensor_copy(out=out, in_=in_), reads=reads, writes=writes)

        def vcol(l, off, n=1):
            b = l * VL + off
            return vecs[:, b:b + n]

        G1, G2, BM, PSC, SUBL, LAM = 0, 8, 16, 64, 68, 69

        sTb = sb("sTb", [128, KC, NB + 1], BF16)

        def norm_mod(l, bcol, Aten, shift_which, xin, n, hout, rx, rh, hf_out=None):
            bi = bank()
            for k in range(KC):
                act(qsq(k, n), xin(k), AF.Square, [rx[k]], [R_sq[k % 2]])
                mm(pb[bi][:, 0:n], ones_bf[:, :], qsq(k, n), k == 0, k == KC - 1, [R_sq[k % 2], R_misc], [Rpb[bi]])
            rs, rrs = RS()
            act(rs[:, 0:n], pb[bi][:, 0:n], AF.Sqrt, [Rpb[bi], R_misc], [rrs], bias=epsc[:, 0:1], scale=1.0 / D)
            P.op("dve", lambda e: e.reciprocal(out=rs[:, 0:n], in_=rs[:, 0:n]), reads=[rrs], writes=[rrs])
            for k in range(KC):
                tq, rq = T()
                tt("dve", tq[:, 0:n], xin(k), rs[:, 0:n], ALU.mult, [rx[k], rrs], [rq])
                act(hout(k), tq[:, 0:n], AF.Identity, [rq, R_mod], [rh[k]],
                    bias=modT[:, l, shift_which * 8 + k, bcol:bcol + 1], scale=Aten[:, l, k, bcol:bcol + 1])
                if hf_out is not None:
                    hf_out(k, tq, rq)

        sqb = [sb(f"sqb{i}", [128, 512], BF16) for i in range(2)]
        rsb = [sb(f"rsb{i}", [128, 512], F32) for i in range(2)]
        R_rsb = [R("rsb0"), R("rsb1")]
        rs_ctr = [0]

        def RS():
            i = rs_ctr[0] % 2
            rs_ctr[0] += 1
            return rsb[i], R_rsb[i]

        R_sq = [R("sq0"), R("sq1")]
        epsc = sb("epsc", [128, 1], F32)

        def qsq(k, n):
            return sqb[k % 2][:, 0:n]

        def rope_store(src_bank, n, t0, dst, rdst, scale):
            i = bank_ctr[0] % 2
            q, rq = sqb[i], R_sq[i]
            act(q[:, 0:n], pb[src_bank][:, 0:n], AF.Copy, [Rpb[src_bank]], [rq], scale=scale)
            b2 = bank()
            mm(pb[b2][:, 0:n], rmat[:, :], q[:, 0:n], True, True, [rq, R_misc], [Rpb[b2]])
            t1, r1 = T()
            tt("dve", t1[:, 0:n], q[:, 0:n], cosT[:, t0:t0 + n], ALU.mult, [rq, R_const], [r1])
            t2, r2 = T()
            tt("dve", t2[:, 0:n], pb[b2][:, 0:n], sinT[:, t0:t0 + n], ALU.mult, [Rpb[b2], R_const], [r2])
            tt("pool", dst, t1[:, 0:n], t2[:, 0:n], ALU.add, [r1, r2], [rdst])

        def xs_tile(par, b, ti):
            return xs_d.ap()[par, b, ti]

        def key(b, ti):
            return (b, ti)

        def load_xT(par, b, ti, n):
            src = xs_tile(par, b, ti).rearrange("p (k t) -> p k t", k=KC)[:, :, 0:n]
            P.dma("sp", sem_xT, xT[:, :, 0:n], src, reads=[R_xs[(par, b, ti)]], writes=R_xk + [R_xT])

        def store_xT(par, b, ti, n):
            dst = xs_tile(par, b, ti).rearrange("p (k t) -> p k t", k=KC)[:, :, 0:n]
            r = R_xs.setdefault((par, b, ti), R(f"xs{par}_{b}_{ti}"))
            P.dma("sp", sem_sp, dst, xT[:, :, 0:n], reads=R_xk + [R_xT], writes=[r])

        def phaseA0_tile(b, ti):
            n = C if ti == 0 else 512
            nblk = n // 128
            for blk in range(nblk):
                i = blk % 2
                if ti == 0:
                    src = ctx_d.ap()[b, blk * 128:(blk + 1) * 128, :]
                else:
                    t0 = (ti - 1) * 512 + blk * 128
                    src = x_d.ap()[b, t0:t0 + 128, :]
                P.dma("sp", sem_x[i], xtm[i], src, writes=R_hid[4 * i:4 * i + 4])
                for k in range(KC):
                    bi = bank()
                    mm(pb[bi][:, 0:128], xtm[i][:, k * 128:(k + 1) * 128], ident[:, :], True, True,
                       R_hid[4 * i:4 * i + 4] + [R_const], [Rpb[bi]])
                    cp("dve" if k % 2 else "act", xT[:, k, blk * 128:(blk + 1) * 128], pb[bi][:, 0:128], [Rpb[bi]], [R_xk[k]])
            store_xT(0, b, ti, n)
            if b == 0 and ti == 1:
                dump("xT0", xT[:, :, :], [128, KC, 512], F32, R_xk)

        def phaseA_tile(l, b, ti, from_sbuf):
            par = l % 2
            n = C if ti == 0 else 512
            kb = 0 if ti == 0 else C + (ti - 1) * 512
            bcol = NB if ti == 0 else b
            if not from_sbuf:
                load_xT(par, b, ti, n)
            norm_mod(l, bcol, A1, 0, lambda k: xT[:, k, 0:n], n, lambda k: hT[:, k, 0:n], R_xk, R_hk)
            if b == 0 and ti == 1:
                dump(f"hT_l{l}", hT[:, :, :], [128, KC, 512], BF16, R_hk)
            if b == 0 and ti == 0 and l == 0:
                dump(f"hcT_l{l}", hT[:, :, 0:C], [128, KC, C], BF16, R_hk)
            wap = w_in_d.ap()[l].rearrange("(k p) n -> p k n", p=128)
            for cb in range(2):
                wv, rw = wload(wap[:, :, OFF_K + cb * 512:OFF_K + (cb + 1) * 512], KC, 512, ("win", l, OFF_K + cb * 512))
                for hh_ in range(4):
                    h = cb * 4 + hh_
                    bi = bank()
                    for k in range(KC):
                        mm(pb[bi][:, 0:n], wv[:, k, hh_ * 128:(hh_ + 1) * 128], hT[:, k, 0:n], k == 0, k == KC - 1,
                           [rw, R_hk[k]], [Rpb[bi]])
                    rk = R_KT.setdefault((h, ti), R(f"KT{h}_{ti}"))
                    if ti == 0:
                        cp("act" if h % 2 else "dve", KT[:, h, kb:kb + n], pb[bi][:, 0:n], [Rpb[bi]], [rk])
                    else:
                        rope_store(bi, n, (ti - 1) * 512, KT[:, h, kb:kb + n], rk, 1.0)
            for cb in range(2):
                wv, rw = wload(wap[:, :, OFF_V + cb * 512:OFF_V + (cb + 1) * 512], KC, 512, ("win", l, OFF_V + cb * 512))
                for tb in range(n // 128):
                    bi = bank()
                    for k in range(KC):
                        mm(pb[bi][:, :], hT[:, k, tb * 128:(tb + 1) * 128], wv[:, k, :], k == 0, k == KC - 1,
                           [rw, R_hk[k]], [Rpb[bi]])
                    kc = kb // 128 + tb
                    rv = R_V.setdefault((kc, cb), R(f"V{kc}_{cb}"))
                    cp("act" if tb % 2 else "dve", Vt[:, kc, cb * 512:(cb + 1) * 512], pb[bi][:, :], [Rpb[bi]], [rv])

        def phaseB_tile(l, b, ti):
            par = l % 2
            last = (l == depth - 1)
            n = C if ti == 0 else 512
            bcol = NB if ti == 0 else b
            qpos = (ti - 1) * 512
            wap = w_in_d.ap()[l].rearrange("(k p) n -> p k n", p=128)
            load_xT(par, b, ti, n)
            has_l = ti >= 2
            has_r = (ti >= 1) and (ti < NT)
            if has_l:
                src = xs_tile(par, b, ti - 1).rearrange("p (k t) -> p k t", k=KC)[:, :, 512 - HAL:512]
                P.dma("sp", sem_xh, xh[:, :, 0:HAL], src, reads=[R_xs[(par, b, ti - 1)]], writes=[R_xh])
            if has_r:
                src = xs_tile(par, b, ti + 1).rearrange("p (k t) -> p k t", k=KC)[:, :, 0:HAL]
                P.dma("sp", sem_xh, xh[:, :, HAL:2 * HAL], src, reads=[R_xs[(par, b, ti + 1)]], writes=[R_xh])
            norm_mod(l, bcol, A1, 0, lambda k: xT[:, k, 0:n], n, lambda k: hT[:, k, 0:n], R_xk, R_hk)
            h0 = 0 if has_l else HAL
            h1 = 2 * HAL if has_r else HAL
            if has_l or has_r:
                if not has_l:
                    P.op("pool", lambda e: e.memset(xh[:, :, 0:HAL], 1.0), writes=[R_xh])
                if not has_r:
                    P.op("pool", lambda e: e.memset(xh[:, :, HAL:2 * HAL], 1.0), writes=[R_xh])
                norm_mod(l, bcol, A1, 0, lambda k: xh[:, k, :], 16, lambda k: hh[:, k, :], [R_xh] * KC, [R_hh] * KC)

            wv, rw = wload(wap[:, :, OFF_POOL:OFF_POOL + 512], KC, 512, ("win", l, OFF_POOL))
            for g in range(4):
                bi = bank()
                for k in range(KC):
                    mm(pb[bi][:, 0:n], wv[:, k, g * 128:(g + 1) * 128], hT[:, k, 0:n], k == 0, k == KC - 1,
                       [rw, R_hk[k]], [Rpb[bi]])
                cp("act", upad[:, g, HAL:HAL + n], pb[bi][:, 0:n], [Rpb[bi]], [R_up[g]])
                if has_l or has_r:
                    b2 = bank()
                    for k in range(KC):
                        mm(pb[b2][:, 0:16], wv[:, k, g * 128:(g + 1) * 128], hh[:, k, :], k == 0, k == KC - 1,
                           [rw, R_hh], [Rpb[b2]])
                    if has_l:
                        cp("dve", upad[:, g, 0:HAL], pb[b2][:, 0:HAL], [Rpb[b2]], [R_up[g]])
                    if has_r:
                        cp("dve", upad[:, g, HAL + n:HAL + n + HAL], pb[b2][:, HAL:2 * HAL], [Rpb[b2]], [R_up[g]])
                if not has_l:
                    P.op("pool", lambda e, g=g: e.memset(upad[:, g, 0:HAL], 0.0), writes=[R_up[g]])
                if not has_r:
                    P.op("pool", lambda e, g=g: e.memset(upad[:, g, HAL + n:HAL + n + HAL], 0.0), writes=[R_up[g]])
            W = n + 2 * HAL
            for g in range(4):
                cur, rcur = upad[:, g, :], R_up[g]
                lo, hi = 0, W
                sh = 0
                for step in range(g + 1):
                    tn, rn = T()
                    eng = "pool" if (g + step) % 2 else "dve"
                    if step == 0:
                        tt(eng, tn[:, 1:W], cur[:, 0:W - 1], cur[:, 1:W], ALU.add, [rcur], [rn])
                        lo, hi = 1, W
                    else:
                        d = 1 << (step - 1)
                        tt(eng, tn[:, lo + d:hi - d], cur[:, lo:hi - 2 * d], cur[:, lo + 2 * d:hi], ALU.add, [rcur], [rn])
                        lo, hi = lo + d, hi - d
                    cur, rcur = tn, rn
                assert lo <= HAL and hi >= HAL + n
                w = POOLW[g]
                stt("dve", AY[:, g, 0:n], cur[:, HAL:HAL + n], 1.0 / w, upad[:, g, HAL:HAL + n], ALU.mult, ALU.subtract,
                    [rcur, R_up[g]], [R_AY[g]])
                if not has_l:
                    te, re_ = T()
                    tt("dve", te[:, 0:HAL], cur[:, HAL:2 * HAL], edge[:, g, 0:HAL], ALU.mult, [rcur, R_const], [re_])
                    tt("dve", AY[:, g, 0:HAL], te[:, 0:HAL], upad[:, g, HAL:2 * HAL], ALU.subtract, [re_, R_up[g]], [R_AY[g]])
                if not has_r:
                    te, re_ = T()
                    tt("dve", te[:, 0:HAL], cur[:, n:n + HAL], edge[:, g, HAL:2 * HAL], ALU.mult, [rcur, R_const], [re_])
                    tt("dve", AY[:, g, n - HAL:n], te[:, 0:HAL], upad[:, g, n:n + HAL], ALU.subtract, [re_, R_up[g]], [R_AY[g]])
                bi = bank()
                mm(pb[bi][:, 0:n], poolw_l(l)[:, g, :], AY[:, g, 0:n], True, True, [R_AY[g], R_pw[l]], [Rpb[bi]])
                act(PY[:, g, 0:n], pb[bi][:, 0:n], AF.Copy, [Rpb[bi], R_const], [R_PY[g]], scale=vcol(l, PSC + g))

            if b == 0 and ti == 1:
                dump(f"PY_l{l}", PY[:, :, :], [128, 4, 512], BF16, R_PY)
            for cb in range(2):
                wv, rw = wload(wap[:, :, OFF_Q + cb * 512:OFF_Q + (cb + 1) * 512], KC, 512, ("win", l, OFF_Q + cb * 512))
                for hh_ in range(4):
                    h = cb * 4 + hh_
                    bi = bank()
                    for k in range(KC):
                        mm(pb[bi][:, 0:n], wv[:, k, hh_ * 128:(hh_ + 1) * 128], hT[:, k, 0:n], k == 0, k == KC - 1,
                           [rw, R_hk[k]], [Rpb[bi]])
                    if ti == 0:
                        act(QT[:, h, 0:n], pb[bi][:, 0:n], AF.Copy, [Rpb[bi]], [R_QT[h]], scale=0.125)
                    else:
                        rope_store(bi, n, qpos, QT[:, h, 0:n], R_QT[h], 0.125)

            if b == 0 and ti == 1:
                dump(f"QT_l{l}", QT[:, :, :], [128, NH, 512], BF16, R_QT)
            kcs = list(range(0, C // 128)) if ti == 0 else list(range(0, NKC))

            def kt_res(h, kc):
                t = 0 if kc < C // 128 else 1 + (kc * 128 - C) // 512
                return R_KT[(h, t)]

            for h in range(NH):
                def S_mm(j, kc):
                    for m in range(2):
                        bi = (j % 2) * 2 + m
                        mm(pb[bi][:, 0:n], KT[64 * m:64 * m + 64, h, kc * 128:(kc + 1) * 128],
                           QT[64 * m:64 * m + 64, h, 0:n], True, True, [kt_res(h, kc), R_QT[h]], [Rpb[bi]])
                S_mm(0, kcs[0])
                for j, kc in enumerate(kcs):
                    if j + 1 < len(kcs):
                        S_mm(j + 1, kcs[j + 1])
                    for m in range(2):
                        bi = (j % 2) * 2 + m
                        act(PT[bi][:, 0:n], pb[bi][:, 0:n], AF.Exp, [Rpb[bi]], [R_PT[bi]])
                    for m in range(2):
                        bi = (j % 2) * 2 + m
                        mm(pb[4 + m][:, 0:n], Vt[:, kc, h * 128:(h + 1) * 128], PT[bi][:, 0:n], j == 0, j == len(kcs) - 1,
                           [R_V[(kc, h // 4)], R_PT[bi]], [Rpb[4 + m]])
                        mm(pb[6 + m][:, 0:n], ones_bf[:, :], PT[bi][:, 0:n], j == 0, j == len(kcs) - 1,
                           [R_misc, R_PT[bi]], [Rpb[6 + m]])
                r1, rr1 = T()
                P.op("dve", lambda e, r1=r1: e.reciprocal(out=r1[:, 0:n], in_=pb[6][:, 0:n]), reads=[Rpb[6]], writes=[rr1])
                r2, rr2 = T()
                P.op("dve", lambda e, r2=r2: e.reciprocal(out=r2[:, 0:n], in_=pb[7][:, 0:n]), reads=[Rpb[7]], writes=[rr2])
                o1, ro1 = T()
                tt("dve", o1[:, 0:n], pb[4][:, 0:n], r1[:, 0:n], ALU.mult, [Rpb[4], rr1], [ro1])
                tt("dve", r2[:, 0:n], pb[5][:, 0:n], r2[:, 0:n], ALU.mult, [Rpb[5], rr2], [rr2])
                stt("dve", AY[:, h, 0:n], r2[:, 0:n], lamv[:, l, 0:1], o1[:, 0:n], ALU.mult, ALU.add, [rr2, ro1, R_mod], [R_AY[h]])
            for h in range(NH):
                i = h % 2
                act(sqb[i][:, 0:n], AY[:, h, 0:n], AF.Square, [R_AY[h]], [R_sq[i]])
                bi = bank()
                mm(pb[bi][:, 0:n], ones_bf[:, :], sqb[i][:, 0:n], True, True, [R_sq[i], R_misc], [Rpb[bi]])
                r1, rr1 = T()
                act(r1[:, 0:n], pb[bi][:, 0:n], AF.Sqrt, [Rpb[bi], R_misc], [rr1], bias=epsc[:, 0:1], scale=1.0 / 128)
                P.op("dve", lambda e, r1=r1: e.reciprocal(out=r1[:, 0:n], in_=r1[:, 0:n]), reads=[rr1], writes=[rr1])
                stt("dve", AY[:, h, 0:n], AY[:, h, 0:n], lamv[:, l, 1:2], r1[:, 0:n], ALU.mult, ALU.mult, [R_AY[h], rr1, R_mod], [R_AY[h]])

            if b == 0 and ti == 1:
                dump(f"AY_l{l}", AY[:, :, :], [128, NH, 512], BF16, R_AY)
            for half in range(2):
                for gsel_ in range(2):
                    wg, rwg = wload(wap[:, :, OFF_G + gsel_ * D + half * 512:OFF_G + gsel_ * D + (half + 1) * 512], KC, 512, ("win", l, OFF_G + gsel_ * D + half * 512))
                    for cc in range(4):
                        bi = bank()
                        for k in range(KC):
                            mm(pb[bi][:, 0:n], wg[:, k, cc * 128:(cc + 1) * 128], hT[:, k, 0:n], k == 0, k == KC - 1,
                               [rwg, R_hk[k]], [Rpb[bi]])
                        act(hid[:, gsel_ * 4 + cc, 0:n], pb[bi][:, 0:n], AF.Sigmoid, [Rpb[bi]], [R_hid[gsel_ * 4 + cc]])
                wpp, rwpp = wload(w_pp_d.ap()[l].rearrange("(k p) n -> p k n", p=128)[:, :, half * 512:(half + 1) * 512], 4, 512, ("wpp", l, half))
                wat, rwat = wload(w_ap_d.ap()[l].rearrange("(k p) n -> p k n", p=128)[:, :, half * 512:(half + 1) * 512], KC, 512, ("wap", l, half))
                for cc in range(4):
                    c = half * 4 + cc
                    b_pp, b_ap = bank(), bank()
                    for k in range(4):
                        mm(pb[b_pp][:, 0:n], wpp[:, k, cc * 128:(cc + 1) * 128], PY[:, k, 0:n], k == 0, k == 3,
                           [rwpp, R_PY[k]], [Rpb[b_pp]])
                    for k in range(KC):
                        mm(pb[b_ap][:, 0:n], wat[:, k, cc * 128:(cc + 1) * 128], AY[:, k, 0:n], k == 0, k == KC - 1,
                           [rwat, R_AY[k]], [Rpb[b_ap]])
                    t1, rt1 = T()
                    tt("dve", t1[:, 0:n], pb[b_pp][:, 0:n], hid[:, cc, 0:n], ALU.mult, [Rpb[b_pp], R_hid[cc]], [rt1])
                    t2, rt2 = T()
                    tt("dve", t2[:, 0:n], pb[b_ap][:, 0:n], hid[:, 4 + cc, 0:n], ALU.mult, [Rpb[b_ap], R_hid[4 + cc]], [rt2])
                    tt("pool", QT[:, c, 0:n], t1[:, 0:n], t2[:, 0:n], ALU.add, [rt1, rt2], [R_QT[c]])
            if b == 0 and ti == 1:
                dump(f"YM_l{l}", QT[:, :, :], [128, NH, 512], BF16, R_QT)
            for half in range(2):
                wv, rw = wload(w_out_d.ap()[l].rearrange("(k p) n -> p k n", p=128)[:, :, half * 512:(half + 1) * 512], KC, 512, ("wout", l, half))
                for cc in range(4):
                    c = half * 4 + cc
                    bi = bank()
                    for k in range(KC):
                        mm(pb[bi][:, 0:n], wv[:, k, cc * 128:(cc + 1) * 128], QT[:, k, 0:n], k == 0, k == KC - 1,
                           [rw, R_QT[k]], [Rpb[bi]])
                    stt("dve", xT[:, c, 0:n], pb[bi][:, 0:n], modT[:, l, 16 + c, bcol:bcol + 1], xT[:, c, 0:n], ALU.mult, ALU.add,
                        [Rpb[bi], R_mod, R_xk[c]], [R_xk[c]])

            if b == 0 and ti == 1:
                dump(f"xmix_l{l}", xT[:, :, :], [128, KC, 512], F32, R_xk)
            if l % 2 == 0:
                norm_mod(l, bcol, A2, 3, lambda k: xT[:, k, 0:n], n, lambda k: hT[:, k, 0:n], R_xk, R_hk)
                i_l = l // 2
                w1ap = ffn_w1_d.ap()[i_l].rearrange("(k p) n -> p k n", p=128)
                w3ap = ffn_w3_d.ap()[i_l].rearrange("(k p) n -> p k n", p=128)
                ffn_half(l, bcol, n, w1ap, w3ap, ffn_w2_d.ap()[i_l], 0, 11 * 128, None, ("ffn", i_l))
                ffn_half(l, bcol, n, w1ap, w3ap, ffn_w2_d.ap()[i_l], 11 * 128, DFF, None, ("ffn", i_l))
            else:
                i_l = l // 2
                moe(l, i_l, bcol, n)

            if b == 0 and ti == 1:
                dump(f"xffn_l{l}", xT[:, :, :], [128, KC, 512], F32, R_xk)
            if last:
                if ti >= 1:
                    final_out(b, ti, n)
            else:
                store_xT(1 - par, b, ti, n)

        def ffn_half(l, bcol, n, w1ap, w3ap, w2ap, f0, f1, gate_bc, wkey):
            nf = f1 - f0
            nch = (nf + 127) // 128
            ci = 0
            for c0 in range(0, nf, 512):
                cw = min(512, nf - c0)
                w1v, rw1 = wload(w1ap[:, :, f0 + c0:f0 + c0 + cw], KC, cw, ("w1", wkey, f0 + c0))
                w3v, rw3 = wload(w3ap[:, :, f0 + c0:f0 + c0 + cw], KC, cw, ("w3", wkey, f0 + c0))
                for cc in range((cw + 127) // 128):
                    m = min(128, cw - cc * 128)
                    b1, b3 = bank(), bank()
                    for k in range(KC):
                        mm(pb[b1][0:m, 0:n], w1v[:, k, cc * 128:cc * 128 + m], hT[:, k, 0:n], k == 0, k == KC - 1,
                           [rw1, R_hk[k]], [Rpb[b1]])
                    for k in range(KC):
                        mm(pb[b3][0:m, 0:n], w3v[:, k, cc * 128:cc * 128 + m], hT[:, k, 0:n], k == 0, k == KC - 1,
                           [rw3, R_hk[k]], [Rpb[b3]])
                    gi = 2 + ci % 2
                    act(gbf[gi][0:m, 0:n], pb[b1][0:m, 0:n], AF.Silu, [Rpb[b1]], [R_gbf[gi]])
                    if gate_bc is None:
                        tt("dve", hid[0:m, ci, 0:n], pb[b3][0:m, 0:n], gbf[gi][0:m, 0:n], ALU.mult, [Rpb[b3], R_gbf[gi]], [R_hid[ci]])
                    else:
                        t1, rt1 = T()
                        tt("dve", t1[0:m, 0:n], pb[b3][0:m, 0:n], gbf[gi][0:m, 0:n], ALU.mult,
                           [Rpb[b3], R_gbf[gi]], [rt1])
                        tt("pool", hid[0:m, ci, 0:n], t1[0:m, 0:n], gate_bc[0][0:m, 0:n], ALU.mult,
                           [rt1, gate_bc[1]], [R_hid[ci]])
                    ci += 1
            assert ci == nch
            w2views = []
            r0 = f0
            while r0 < f1:
                rows = min(512, f1 - r0)
                nk = rows // 128
                rem = rows - nk * 128
                s_ = w_ctr[0] % cfg.NW
                w_ctr[0] += 1
                nkk = nk + (1 if rem else 0)
                flat = wsl[s_][:, 0:nkk * D]
                view = flat.rearrange("p (k c) -> p k c", k=nkk)
                key = ("w2", wkey, r0)
                if key in wcache:
                    scr, rscr = wcache[key]
                    P.dma("sp", wsem[s_], flat, scr.ap(), reads=[rscr], writes=[Rw[s_]])
                else:
                    if nk > 0:
                        P.dma("pool", wsem[s_], view[:, 0:nk, :], w2ap[r0:r0 + nk * 128, :].rearrange("(k p) n -> p k n", p=128),
                              writes=[Rw[s_]])
                    if rem:
                        P.dma("pool", wsem[s_], view[0:rem, nk, :], w2ap[r0 + nk * 128:r0 + rows, :], writes=[Rw[s_]])
                    if getattr(cfg, "wcache", True):
                        scr = nc.dram_tensor(f"ws{len(wcache)}", [128, nkk * D], BF16)
                        rscr = R(f"ws{len(wcache)}")
                        P.dma("sp", ssem[s_], scr.ap(), flat, reads=[Rw[s_]], writes=[rscr])
                        wcache[key] = (scr, rscr)
                for k in range(nk):
                    w2views.append((view, k, 128, Rw[s_]))
                if rem:
                    w2views.append((view, nk, rem, Rw[s_]))
                r0 += rows
            assert len(w2views) == nch
            for c in range(KC):
                bi = bank()
                for j, (v, k, m, rw) in enumerate(w2views):
                    mm(pb[bi][:, 0:n], v[0:m, k, c * 128:(c + 1) * 128], hid[0:m, j, 0:n], j == 0, j == nch - 1,
                       [rw, R_hid[j]], [Rpb[bi]])
                stt("dve", xT[:, c, 0:n], pb[bi][:, 0:n], modT[:, l, 40 + c, bcol:bcol + 1], xT[:, c, 0:n], ALU.mult, ALU.add,
                    [Rpb[bi], R_mod, R_xk[c]], [R_xk[c]])

        moe_dumped = []

        def moe(l, i_l, bcol, n):
            nblk = n // 128
            lgb = bank()

            def hf_out(k, tq, rq):
                hf, rhf = T()
                act(hf[:, 0:n], tq[:, 0:n], AF.Identity, [rq, R_mod], [rhf],
                    bias=modT[:, l, 3 * 8 + k, bcol:bcol + 1], scale=A2[:, l, k, bcol:bcol + 1])
                i = k % 4
                tt("dve", PT[i][:, 0:n], hf[:, 0:n], hT[:, k, 0:n], ALU.subtract, [rhf, R_hk[k]], [R_PT[i]])
                mm(pb[lgb][0:8, 0:n], wrh[:, k, :], hT[:, k, 0:n], k == 0, False, [R_misc, R_hk[k]], [Rpb[lgb]])
                mm(pb[lgb][0:8, 0:n], wrh[:, k, :], PT[i][:, 0:n], False, False, [R_misc, R_PT[i]], [Rpb[lgb]])
                mm(pb[lgb][0:8, 0:n], wrl[:, k, :], hT[:, k, 0:n], False, k == KC - 1, [R_misc, R_hk[k]], [Rpb[lgb]])
            norm_mod(l, bcol, A2, 3, lambda k: xT[:, k, 0:n], n, lambda k: hT[:, k, 0:n], R_xk, R_hk, hf_out=hf_out)
            lgT, rlgT = T()
            cp("dve", lgT[0:8, 0:n], pb[lgb][0:8, 0:n], [Rpb[lgb]], [rlgT])
            cp("dve", gsel[0][:, 0:n], lgT[0:8, 0:n], [rlgT], [R_gs[0]])
            tt("dve", gsel[1][:, 0:n], lgT[0:8, 0:n], gsel[0][:, 0:n], ALU.subtract, [rlgT, R_gs[0]], [R_gs[1]])
            lb2 = bank()
            for tb in range(nblk):
                mm(pb[lb2][:, tb * 8:tb * 8 + 8], gsel[0][0:8, tb * 128:(tb + 1) * 128], identb[0:8, 0:8], True, False,
                   [R_gs[0], R_misc], [Rpb[lb2]])
                mm(pb[lb2][:, tb * 8:tb * 8 + 8], gsel[1][0:8, tb * 128:(tb + 1) * 128], identb[0:8, 0:8], False, True,
                   [R_gs[1], R_misc], [Rpb[lb2]])
            cp("dve", lg[:, 0:nblk, :], pb[lb2][:, 0:nblk * 8].rearrange("p (a e) -> p a e", e=8), [Rpb[lb2]], [R_lg])
            for tb in range(nblk):
                P.op("dve", lambda e, tb=tb: e.max(out=mx8[:, tb, :], in_=lg[:, tb, :]), reads=[R_lg], writes=[R_gate])
            tt("dve", sm[:, 0:nblk], mx8[:, 0:nblk, 1], mx8[:, 0:nblk, 0], ALU.subtract, [R_gate], [R_gate])
            act(sm[:, 4:4 + nblk], sm[:, 0:nblk], AF.Exp, [R_gate], [R_gate])
            ts("dve", sm[:, 4:4 + nblk], sm[:, 4:4 + nblk], 1.0, None, ALU.add, None, [R_gate], [R_gate])
            P.op("dve", lambda e: e.reciprocal(out=sm[:, 8:8 + nblk], in_=sm[:, 4:4 + nblk]), reads=[R_gate], writes=[R_gate])
            ts("dve", sm[:, 12:12 + nblk], sm[:, 8:8 + nblk], -1.0, 1.0, ALU.mult, ALU.add, [R_gate], [R_gate])
            for tb in range(nblk):
                ts("dve", gt[:, tb, :], lg[:, tb, :], mx8[:, tb, 0:1], sm[:, 8 + tb:9 + tb], ALU.is_equal, ALU.mult, [R_lg, R_gate], [R_gate])
                t1, rt1 = T()
                ts("dve", t1[:, 0:8], lg[:, tb, :], mx8[:, tb, 1:2], sm[:, 12 + tb:13 + tb], ALU.is_equal, ALU.mult, [R_lg, R_gate], [rt1])
                tt("dve", gtb[:, tb, :], gt[:, tb, :], t1[:, 0:8], ALU.add, [R_gate, rt1], [R_gate])
            gb = bank()
            for tb in range(nblk):
                mm(pb[gb][0:8, tb * 128:(tb + 1) * 128], gtb[:, tb, :], identb[:, :], True, True, [R_gate, R_misc], [Rpb[gb]])
            cp("act", gT8[:, 0:n], pb[gb][0:8, 0:n], [Rpb[gb]], [R_gate])
            if bcol == 0 and n == 512 and not moe_dumped:
                moe_dumped.append(1)
                dump("lg", lg[:, :, :], [128, 4, NE], F32, [R_lg])
                dump("mx8", mx8[:, :, :], [128, 4, 8], F32, [R_gate])
                dump("gt", gtb[:, :, :], [128, 4, NE], BF16, [R_gate])
                dump("gT8", gT8[:, :], [8, 512], BF16, [R_gate])
            for e in range(NE):
                gb = bank()
                gi = e % 2
                ts("dve", gsel[gi][:, 0:n], gT8[:, 0:n], identf8(e), None, ALU.mult, None, [R_gate, R_const], [R_gs[gi]])
                mm(pb[gb][:, 0:n], ones_bf[0:8, :], gsel[gi][:, 0:n], True, True, [R_gs[gi], R_misc], [Rpb[gb]])
                cp("act", gbf[gi][:, 0:n], pb[gb][:, 0:n], [Rpb[gb]], [R_gbf[gi]])
                w1ap = moe_w1_d.ap()[i_l, e].rearrange("(k p) n -> p k n", p=128)
                w3ap = moe_w3_d.ap()[i_l, e].rearrange("(k p) n -> p k n", p=128)
                ffn_half(l, bcol, n, w1ap, w3ap, moe_w2_d.ap()[i_l, e], 0, DFE, (gbf[gi], R_gbf[gi]), ("moe", i_l, e))

        identb = sb("identb", [128, 128], BF16)
        R_gs = [R("gs0"), R("gs1")]

        def identf8(e):
            return ident[0:8, e:e + 1]

        def final_out(b, ti, n):
            bi = bank()
            for k in range(KC):
                act(qsq(k, n), xT[:, k, 0:n], AF.Square, [R_xk[k]], [R_sq[k % 2]])
                mm(pb[bi][:, 0:n], ones_bf[:, :], qsq(k, n), k == 0, k == KC - 1, [R_sq[k % 2], R_misc], [Rpb[bi]])
            rs, rrs = RS()
            act(rs[:, 0:n], pb[bi][:, 0:n], AF.Sqrt, [Rpb[bi], R_misc], [rrs], bias=epsc[:, 0:1], scale=1.0 / D)
            P.op("dve", lambda e: e.reciprocal(out=rs[:, 0:n], in_=rs[:, 0:n]), reads=[rrs], writes=[rrs])
            for k in range(KC):
                stt("dve", xT[:, k, 0:n], xT[:, k, 0:n], vecs[:, 2 * VL + k:2 * VL + k + 1], rs[:, 0:n], ALU.mult, ALU.mult,
                    [R_xk[k], rrs, R_const], [R_xk[k]])
            for blk in range(n // 128):
                i = blk % 2
                for half in range(2):
                    bi = bank()
                    for kk in range(4):
                        k = half * 4 + kk
                        mm(pb[bi][:, kk * 128:(kk + 1) * 128], xT[:, k, blk * 128:(blk + 1) * 128], ident[:, :], True, True,
                           [R_xk[k], R_const], [Rpb[bi]])
                    cp("act" if half else "dve", xtm[i][:, half * 512:(half + 1) * 512], pb[bi][:, :], [Rpb[bi]], R_hid[4 * i:4 * i + 4])
                t0 = (ti - 1) * 512 + blk * 128
                o = P.dma("sp", sem_o[i], out_d.ap()[b, t0:t0 + 128, :], xtm[i], reads=R_hid[4 * i:4 * i + 4])
                fw_last[i] = (sem_o[i], o.dval)

        fw_last = {}
        R_pw = [R("pw0"), R("pw1")]
        poolw2 = sb("poolw2", [128, 2, 4, 128], BF16)

        def poolw_l(l):
            return poolw2[:, l]

        P.op("dve", lambda e: e.memset(epsc[:, :], EPS), writes=[R_misc])
        def run_prologue():
            P.dma("sp", sem_c, ident[:, :], ident_d.ap()[:, :], writes=[R_const])
            P.dma("sp", sem_c, vecs[:, :], vecs_d.ap()[:, :], writes=[R_const])
            P.dma("pool", sem_sp, cosT[:, :], cos_d.ap()[:, :], writes=[R_const])
            P.dma("pool", sem_sp, sinT[:, :], sin_d.ap()[:, :], writes=[R_const])
            P.dma("sp", sem_c, edge[:, :, :], edge_d.ap()[:, :, :], writes=[R_const])
            P.dma("sp", sem_c, sT[:, :, :], cT_d.ap()[:, :, :], writes=[R_const])
            P.dma("sp", sem_c, wr[:, :, :], router_d.ap()[0].rearrange("(k p) e -> p k e", p=128), writes=[R_const])
            P.dma("pool", sem_sp, rmat[:, :], rmat_d.ap()[:, :], writes=[R_misc])
            P.dma("pool", sem_sp, identb[:, :], ident_d.ap()[:, :], writes=[R_misc])
            for l in range(depth):
                P.dma("pool", sem_sp, poolw2[:, l], pool_w_d.ap()[l].rearrange("g c d -> c g d"), writes=[R_pw[l]])
            P.op("dve", lambda e: e.memset(ones_bf[:, :], 1.0), writes=[R_misc])
            P.op("dve", lambda e: e.memset(ones_f[:, :], 1.0), writes=[R_misc])
            act(sT[:, :, :], sT[:, :, :], AF.Silu, [R_const], [R_const])
            cp("dve", sTb[:, :, :], sT[:, :, :], [R_const], [R_misc])
            cp("dve", wrh[:, :, :], wr[:, :, :], [R_const], [R_misc])
            tt("dve", wrl[:, :, :], wr[:, :, :], wrh[:, :, :], ALU.subtract, [R_const, R_misc], [R_misc])
            nb1 = NB + 1
            for l in range(depth):
                for jb in range(12):
                    wv, rw = wload(w_mod_d.ap()[l].rearrange("(k p) n -> p k n", p=128)[:, :, jb * 512:(jb + 1) * 512], KC, 512)
                    bi = bank()
                    for jj in range(4):
                        for k in range(KC):
                            mm(pb[bi][:, jj * 8:jj * 8 + nb1], wv[:, k, jj * 128:(jj + 1) * 128], sTb[:, k, :],
                               k == 0, k == KC - 1, [rw, R_misc], [Rpb[bi]])
                    for jj in range(4):
                        j = jb * 4 + jj
                        act(modT[:, l, j, :], pb[bi][:, jj * 8:jj * 8 + nb1], AF.Identity, [Rpb[bi], R_const], [R_mod],
                            bias=vcol(l, BM + j), scale=1.0)
                for k in range(KC):
                    ts("dve", A1[:, l, k, :], modT[:, l, 8 + k, :], 1.0, vcol(l, G1 + k), ALU.add, ALU.mult, [R_mod, R_const], [R_mod])
                    ts("dve", A2[:, l, k, :], modT[:, l, 32 + k, :], 1.0, vcol(l, G2 + k), ALU.add, ALU.mult, [R_mod, R_const], [R_mod])
                t0, r0 = T()
                tt("dve", t0[0:64, 0:1], vcol(l, LAM + 0)[0:64, :], vcol(l, LAM + 1)[0:64, :], ALU.mult, [R_const], [r0])
                tt("dve", t0[0:64, 1:2], vcol(l, LAM + 2)[0:64, :], vcol(l, LAM + 3)[0:64, :], ALU.mult, [R_const], [r0])
                bi = bank()
                cp("dve", sqb[0][0:64, 0:2], t0[0:64, 0:2], [r0], [R_sq[0]])
                tt("dve", sqb[0][0:64, 2:4], t0[0:64, 0:2], sqb[0][0:64, 0:2], ALU.subtract, [r0, R_sq[0]], [R_sq[0]])
                mm(pb[bi][:, 0:2], ones_bf[0:64, :], sqb[0][0:64, 0:2], True, False, [R_sq[0], R_misc], [Rpb[bi]])
                mm(pb[bi][:, 0:2], ones_bf[0:64, :], sqb[0][0:64, 2:4], False, True, [R_sq[0], R_misc], [Rpb[bi]])
                t1, r1 = T()
                act(t1[:, 0:2], pb[bi][:, 0:2], AF.Exp, [Rpb[bi]], [r1])
                lam0 = lambda_init(l)
                tt("dve", t1[:, 2:3], t1[:, 1:2], t1[:, 0:1], ALU.subtract, [r1], [r1])
                ts("dve", lamv[:, l, 0:1], t1[:, 2:3], -lam0, None, ALU.add, None, [r1], [R_mod])
                ts("dve", lamv[:, l, 1:2], vcol(l, SUBL), 1.0 - lam0, None, ALU.mult, None, [R_const], [R_mod])

        run_prologue()
        dump("modT", modT[:, :, :, :], [128, 2, 48, NB + 1], F32, [R_mod])
        dump("lamv", lamv[:, :, :], [128, 2, 4], F32, [R_mod])
        dump("A1", A1[:, :, :, :], [128, 2, KC, NB + 1], F32, [R_mod])

        for b in range(NB):
            for ti in range(NT + 1):
                phaseA0_tile(b, ti)
                phaseA_tile(0, b, ti, from_sbuf=True)
            for l in range(depth):
                last = (l == depth - 1)
                if l > 0:
                    for ti in range(NT + 1):
                        phaseA_tile(l, b, ti, from_sbuf=False)
                tiles = list(range(1, NT + 1)) if last else list(range(0, NT + 1))
                if b == 0:
                    dump(f"KT_l{l}", KT[:, :, :], [128, NH, S], BF16, list(R_KT.values()))
                    dump(f"V_l{l}", Vt[:, :, :], [128, NKC, D], BF16, list(R_V.values()))
                for ti in tiles:
                    phaseB_tile(l, b, ti)

        for i in fw_last:
            final_waits.append(fw_last[i])
        P.emit(mksem, final_waits)
    return nc, P


def host_consts(L):
    ident = np.eye(128, dtype=np.float32)
    p = np.arange(128)
    j = p % 64
    axis = j // 32
    ab = (j % 32) // 16
    pair = j % 16
    partner = np.where(ab == 0, p + 16, p - 16)
    rmat = np.zeros((128, 128), np.float32)
    rmat[partner, p] = 1.0
    inv = (10000.0 ** (-np.arange(16, dtype=np.float32) * 2.0 / 32)).astype(np.float32)
    t = np.arange(L)
    pos = np.stack([(t // GRID_W).astype(np.float32), (t % GRID_W).astype(np.float32)], 0)
    ang = pos[axis] * inv[pair][:, None]
    cosT = np.cos(ang).astype(np.float32)
    sgn = np.where(ab == 0, -1.0, 1.0).astype(np.float32)[:, None]
    sinT = (np.sin(ang).astype(np.float32) * sgn).astype(np.float32)
    edge = np.zeros((128, 4, 16), np.float32)
    BIG = 1 << 20
    for g, w in enumerate(POOLW):
        for tt_ in range(8):
            lo = max(tt_ - w // 2, 0)
            hi = tt_ + w // 2
            edge[:, g, tt_] = 1.0 / (hi - lo)
            tpos = BIG - 8 + tt_
            lo = tpos - w // 2
            hi = min(tpos + w // 2, BIG)
            edge[:, g, 8 + tt_] = 1.0 / (hi - lo)
    sel = np.zeros((8, 8 * 128), np.float32)
    for e in range(8):
        sel[e, e * 128:(e + 1) * 128] = 1.0
    return ident, rmat, cosT, sinT, edge, sel


def pack_vecs(inp):
    v = np.zeros((128, NV), np.float32)
    for l in range(2):
        b = l * VL
        v[:, b + 0:b + 8] = inp["norm1_g"][l].reshape(8, 128).T
        v[:, b + 8:b + 16] = inp["norm2_g"][l].reshape(8, 128).T
        v[:, b + 16:b + 64] = inp["b_mod"][l].reshape(48, 128).T
        v[:, b + 64:b + 68] = inp["pool_scale"][l].reshape(4, 128).T
        v[:, b + 68] = inp["subln_g"][l]
        v[0:64, b + 69] = inp["lam_q1"][l]
        v[0:64, b + 70] = inp["lam_k1"][l]
        v[0:64, b + 71] = inp["lam_q2"][l]
        v[0:64, b + 72] = inp["lam_k2"][l]
    v[:, 2 * VL:2 * VL + 8] = inp["final_g"].reshape(8, 128).T
    return v


_CACHE = {}


def run(inp, cfg, n_cores):
    keyc = (cfg.NB, cfg.L, cfg.C, cfg.depth, cfg.NW)
    if keyc not in _CACHE:
        _CACHE[keyc] = build(cfg)
    nc, P = _CACHE[keyc]
    ident, rmat, cosT, sinT, edge, sel = host_consts(cfg.L)
    vecs = pack_vecs(inp)
    f = lambda a: np.ascontiguousarray(np.asarray(a, dtype=np.float32))
    shared = {
        "vecs": vecs, "ident": ident, "rmat": rmat, "cosT": cosT, "sinT": sinT, "edge": edge, "sel": sel,
        "w_mod": f(inp["w_mod"]), "w_in": f(inp["w_in"]), "pool_w": f(inp["pool_w"]),
        "w_pool_proj": f(inp["w_pool_proj"]), "w_attn_proj": f(inp["w_attn_proj"]), "w_out": f(inp["w_out"]),
        "ffn_w1": f(inp["ffn_w1"]), "ffn_w3": f(inp["ffn_w3"]), "ffn_w2": f(inp["ffn_w2"]),
        "router_w": f(inp["router_w"]), "moe_w1": f(inp["moe_w1"]), "moe_w3": f(inp["moe_w3"]), "moe_w2": f(inp["moe_w2"]),
    }
    NB = cfg.NB
    in_maps = []
    x = f(inp["x"])
    ctx = f(inp["ctx"])
    c = f(inp["c"])
    c_ctx = f(inp["c_ctx"])
    for i in range(n_cores):
        cc = np.concatenate([c[i * NB:(i + 1) * NB], c_ctx[None, :]], 0)
        cT = np.ascontiguousarray(cc.reshape(NB + 1, 8, 128).transpose(2, 1, 0))
        m = dict(shared)
        m["x"] = np.ascontiguousarray(x[i * NB:(i + 1) * NB])
        m["ctx"] = np.ascontiguousarray(ctx[i * NB:(i + 1) * NB])
        m["cT"] = cT
        in_maps.append(m)
    res = run_bass_kernel_spmd(nc, in_maps, core_ids=list(range(n_cores)))
    if getattr(cfg, "debug", False):
        cfg.dbg = {k: np.asarray(res.results[0][k]) for k in cfg.dbg_names}
    return np.concatenate([r["out"] for r in res.results], axis=0)


def kernel(**inputs):
    cfg = Cfg(NB=4, L=2048, C=256, depth=2, NW=3)
    return run(inputs, cfg, 8)
```

```python
import math
from contextlib import ExitStack
import numpy as np
import concourse.bass as bass
import concourse.mybir as mybir
from concourse.bass_utils import run_bass_kernel_spmd

F32 = mybir.dt.float32
BF16 = mybir.dt.bfloat16
AF = mybir.ActivationFunctionType
ALU = mybir.AluOpType

ENGS = ("pe", "act", "dve", "pool", "sp")
EPOCH = 30000


class Res:
    __slots__ = ("name", "w", "r")

    def __init__(self, name):
        self.name = name
        self.w = None
        self.r = {}


class Op:
    __slots__ = ("eng", "fn", "deps", "pos", "flag", "dsem", "dval", "waits", "ordinal")

    def __init__(self, eng, fn):
        self.eng = eng
        self.fn = fn
        self.deps = []
        self.pos = -1
        self.flag = False
        self.dsem = None
        self.dval = 0
        self.waits = []
        self.ordinal = 0


class Prog:
    def __init__(self, nc):
        self.nc = nc
        self.ops = {e: [] for e in ENGS}
        self.dma_counts = {}
        self.last_dma = {}
        self.nres = 0

    def res(self, name=None):
        self.nres += 1
        return Res(name or f"r{self.nres}")

    def _add(self, op, reads, writes):
        deps = {}
        for r in reads:
            if r.w is not None:
                deps[id(r.w)] = (r.w, True)
        for w in writes:
            if w.w is not None and id(w.w) not in deps:
                deps[id(w.w)] = (w.w, False)
            for o in w.r.values():
                if id(o) not in deps:
                    deps[id(o)] = (o, False)
        for (o, raw) in deps.values():
            if o is op:
                continue
            if o.dsem is None and op.dsem is None and o.eng == op.eng:
                if (not raw) or op.eng == "pe":
                    continue
            op.deps.append(o)
        op.pos = len(self.ops[op.eng])
        self.ops[op.eng].append(op)
        for r in reads:
            r.r[op.eng if op.dsem is None else ("dma", id(op))] = op
        for w in writes:
            w.w = op
            w.r = {}
        return op

    def op(self, eng, fn, reads=(), writes=()):
        return self._add(Op(eng, fn), reads, writes)

    def dma(self, queue, sem, out, in_, reads=(), writes=()):
        def fn(e, out=out, in_=in_):
            return e.dma_start(out=out, in_=in_)
        op = Op(queue, fn)
        op.dsem = sem
        k = self.dma_counts.get(id(sem), 0) + 1
        self.dma_counts[id(sem)] = k
        op.dval = 16 * k
        assert op.dval < 65000
        prev = self.last_dma.get(id(sem))
        self._add(op, reads, writes)
        if prev is not None and all(d is not prev for d in op.deps):
            op.deps.append(prev)
        self.last_dma[id(sem)] = op
        return op

    def finalize(self):
        for e in ENGS:
            waited_pos = {}
            waited_dma = {}
            for op in self.ops[e]:
                for d in op.deps:
                    if d.dsem is not None:
                        if waited_dma.get(id(d.dsem), 0) >= d.dval:
                            continue
                        waited_dma[id(d.dsem)] = d.dval
                        op.waits.append(("dma", d))
                    else:
                        if waited_pos.get(d.eng, -1) >= d.pos:
                            continue
                        waited_pos[d.eng] = d.pos
                        d.flag = True
                        op.waits.append(("eng", d))
        self.nflag = {}
        for e in ENGS:
            n = 0
            for op in self.ops[e]:
                if op.dsem is None and op.flag:
                    op.ordinal = n
                    n += 1
            self.nflag[e] = n

    def emit(self, mksem, final_waits=()):
        nc = self.nc
        self.finalize()
        esems = {}
        for e in ENGS:
            esems[e] = [mksem(f"es_{e}_{i}") for i in range(self.nflag[e] // EPOCH + 1)]
        engobj = {"pe": "tensor", "act": "scalar", "dve": "vector", "pool": "gpsimd", "sp": "sync"}

        def run(e, eng):
            for op in self.ops[e]:
                for kind, d in op.waits:
                    if kind == "dma":
                        eng.wait_ge(d.dsem, d.dval)
                    else:
                        eng.wait_ge(esems[d.eng][d.ordinal // EPOCH], d.ordinal % EPOCH + 1)
                ins = op.fn(eng)
                if op.dsem is not None:
                    ins.then_inc(op.dsem, 16)
                elif op.flag:
                    ins.then_inc(esems[e][op.ordinal // EPOCH], 1)
            if e == "sp":
                for (s, v) in final_waits:
                    eng.wait_ge(s, v)

        with nc.Block() as block:
            for e in ENGS:
                getattr(block, engobj[e])(lambda eng, e=e: run(e, eng))

    def stats(self):
        return {e: len(self.ops[e]) for e in ENGS}


D = 1024
KC = 8
NH = 8
GRID_W = 64
EPS = 1e-6
POOLW = (2, 4, 8, 16)
OFF_POOL, OFF_Q, OFF_K, OFF_V, OFF_G = 0, 512, 1536, 2560, 3584
IN_W = 5632
DFF = 2752
NE = 8
DFE = 1408
VL = 73
NV = 2 * VL + 8
WSLOT = 4096
HAL = 8


def lambda_init(layer):
    return 0.8 - 0.6 * math.exp(-0.3 * layer)


class Cfg:
    def __init__(self, NB=4, L=2048, C=256, depth=2, NW=3):
        self.NB, self.L, self.C, self.depth, self.NW = NB, L, C, depth, NW
        self.TQ = 512
        self.NT = L // 512
        self.S = C + L
        self.NKC = self.S // 128


def build(cfg):
    NB, L, C, NT, S, NKC = cfg.NB, cfg.L, cfg.C, cfg.NT, cfg.S, cfg.NKC
    depth = cfg.depth
    nc = bass.Bass("TRN2", target_bir_lowering=False)

    def din(name, shape):
        return nc.dram_tensor(name, list(shape), F32, kind="ExternalInput")

    x_d = din("x", [NB, L, D])
    ctx_d = din("ctx", [NB, C, D])
    cT_d = din("cT", [128, KC, NB + 1])
    vecs_d = din("vecs", [128, NV])
    ident_d = din("ident", [128, 128])
    rmat_d = din("rmat", [128, 128])
    cos_d = din("cosT", [128, L])
    sin_d = din("sinT", [128, L])
    edge_d = din("edge", [128, 4, 16])
    sel_d = din("sel", [8, 8 * 128])
    w_mod_d = din("w_mod", [2, D, 6 * D])
    w_in_d = din("w_in", [2, D, IN_W])
    pool_w_d = din("pool_w", [2, 4, 128, 128])
    w_pp_d = din("w_pool_proj", [2, 512, D])
    w_ap_d = din("w_attn_proj", [2, D, D])
    w_out_d = din("w_out", [2, D, D])
    ffn_w1_d = din("ffn_w1", [1, D, DFF])
    ffn_w3_d = din("ffn_w3", [1, D, DFF])
    ffn_w2_d = din("ffn_w2", [1, DFF, D])
    router_d = din("router_w", [1, D, NE])
    moe_w1_d = din("moe_w1", [1, NE, D, DFE])
    moe_w3_d = din("moe_w3", [1, NE, D, DFE])
    moe_w2_d = din("moe_w2", [1, NE, DFE, D])
    out_d = nc.dram_tensor("out", [NB, L, D], F32, kind="ExternalOutput")
    xs_d = nc.dram_tensor("xs", [2, NB, NT + 1, 128, KC * 512], F32)

    es = ExitStack()
    with es:
        def sb(name, shape, dt):
            return es.enter_context(nc.sbuf_tensor("sb_" + name, list(shape), dt))

        def mksem(name):
            return es.enter_context(nc.semaphore(name))

        P = Prog(nc)
        R = P.res

        KT = sb("KT", [128, NH, S], BF16)
        Vt = sb("Vt", [128, NKC, D], BF16)
        cosT = sb("cosT", [128, L], BF16)
        sinT = sb("sinT", [128, L], BF16)
        wsl = [sb(f"wsl{i}", [128, WSLOT], BF16) for i in range(cfg.NW)]
        xT = sb("xT", [128, KC, 512], F32)
        xh = sb("xh", [128, KC, 16], F32)
        hT = sb("hT", [128, KC, 512], BF16)
        hh = sb("hh", [128, KC, 16], BF16)
        NTMP = 6
        tmp = [sb(f"tmp{i}", [128, 528], F32) for i in range(NTMP)]
        QT = sb("QT", [128, NH, 512], BF16)
        PT = [sb(f"PT{i}", [128, 512], BF16) for i in range(4)]
        AY = sb("AY", [128, NH, 512], BF16)
        upad = sb("upad", [128, 4, 528], F32)
        PY = sb("PY", [128, 4, 512], BF16)
        hidraw = sb("hidraw", [128, 2816], F32)
        hid = hidraw.bitcast(BF16)[:, 0:5632].rearrange("p (k t) -> p k t", k=11)
        gbf = [sb(f"gbf{i}", [128, 512], BF16) for i in range(4)]
        xtm = [hidraw[:, i * 1024:(i + 1) * 1024] for i in range(2)]
        ident = sb("ident", [128, 128], F32)
        rmat = sb("rmat", [128, 128], BF16)
        ones_bf = sb("ones_bf", [128, 128], BF16)
        ones_f = sb("ones_f", [128, 128], F32)
        vecs = sb("vecs", [128, NV], F32)
        edge = sb("edge", [128, 4, 16], F32)
        gsel = [sb(f"gsel{i}", [8, 512], BF16) for i in range(2)]
        R_gsel = None
        sT = sb("sT", [128, KC, NB + 1], F32)
        modT = sb("modT", [128, 2, 48, NB + 1], F32)
        A1 = sb("A1", [128, 2, KC, NB + 1], F32)
        A2 = sb("A2", [128, 2, KC, NB + 1], F32)
        lamv = sb("lamv", [128, 2, 4], F32)
        wr = sb("wr", [128, KC, NE], F32)
        wrh = sb("wrh", [128, KC, NE], BF16)
        wrl = sb("wrl", [128, KC, NE], BF16)
        lg = sb("lg", [128, 4, NE], F32)
        gt = sb("gt", [128, 4, NE], F32)
        gtb = sb("gtb", [128, 4, NE], BF16)
        gT8 = sb("gT8", [8, 512], BF16)
        mx8 = sb("mx8", [128, 4, 8], F32)
        sm = sb("sm", [128, 16], F32)

        pb = [es.enter_context(nc.psum_tensor(f"pb{i}", [128, 512], F32)) for i in range(8)]
        Rpb = [R(f"pb{i}") for i in range(8)]
        bank_ctr = [0]

        def bank(lo=0, hi=8):
            i = lo + bank_ctr[0] % (hi - lo)
            bank_ctr[0] += 1
            return i

        R_KT = {}
        R_V = {}
        Rw = [R(f"w{i}") for i in range(cfg.NW)]
        wsem = [mksem(f"wsem{i}") for i in range(cfg.NW)]
        R_xT, R_xh, R_hT, R_hh = R("xT"), R("xh"), R("hT"), R("hh")
        R_xk = [R(f"xTk{k}") for k in range(KC)]
        R_hk = [R(f"hTk{k}") for k in range(KC)]
        Rtmp = [R(f"tmp{i}") for i in range(NTMP)]
        R_QT = [R(f"QT{h}") for h in range(NH)]
        R_PT = [R(f"PT{i}") for i in range(4)]
        R_AY = [R(f"AY{h}") for h in range(NH)]
        R_up = [R(f"up{g}") for g in range(4)]
        R_PY = [R(f"PY{g}") for g in range(4)]
        R_hid = [R(f"hid{i}") for i in range(11)]
        R_gbf = [R(f"gbf{i}") for i in range(4)]
        R_const = R("const")
        R_mod = R("mod")
        R_misc = R("misc")
        R_gate = R("gate")
        R_hf = R("hf")
        R_lg = R("lg")
        R_xs = {}
        tmp_ctr = [0]

        def T():
            i = tmp_ctr[0] % NTMP
            tmp_ctr[0] += 1
            return tmp[i], Rtmp[i]

        sem_c = mksem("sem_c")
        sem_x = [mksem(f"sem_x{i}") for i in range(2)]
        sem_xT = mksem("sem_xT")
        sem_xh = mksem("sem_xh")
        sem_o = [mksem(f"sem_o{i}") for i in range(2)]
        sem_sp = mksem("sem_sp")
        final_waits = []
        dbg_sem = mksem("dbg_sem")
        dbg_names = []

        def dump(name, ap, shape, dt, reads):
            if not getattr(cfg, "debug", False):
                return
            if cfg.debug is not True and not any(name.startswith(p) for p in cfg.debug):
                return
            t = nc.dram_tensor("dbg_" + name, list(shape), dt, kind="ExternalOutput")
            o = P.dma("sp", dbg_sem, t.ap(), ap, reads=reads)
            final_waits.append((dbg_sem, o.dval))
            dbg_names.append("dbg_" + name)
        cfg.dbg_names = dbg_names

        w_ctr = [0]

        wcache = {}
        ssem = [mksem(f"ssem{i}") for i in range(cfg.NW)]

        def wload(src_ap, nk, ncols, key=None):
            s = w_ctr[0] % cfg.NW
            w_ctr[0] += 1
            flat = wsl[s][:, 0:nk * ncols]
            view = flat.rearrange("p (k c) -> p k c", k=nk)
            if key is not None and key in wcache:
                scr, rscr = wcache[key]
                P.dma("sp", wsem[s], flat, scr.ap(), reads=[rscr], writes=[Rw[s]])
                return view, Rw[s]
            P.dma("pool", wsem[s], view, src_ap, writes=[Rw[s]])
            if key is not None and getattr(cfg, "wcache", True):
                scr = nc.dram_tensor(f"ws{len(wcache)}", [128, nk * ncols], BF16)
                rscr = R(f"ws{len(wcache)}")
                P.dma("sp", ssem[s], scr.ap(), flat, reads=[Rw[s]], writes=[rscr])
                wcache[key] = (scr, rscr)
            return view, Rw[s]

        def mm(out, lhsT, rhs, start, stop, reads, writes):
            P.op("pe", lambda e: e.matmul(out, lhsT=lhsT, rhs=rhs, start=start, stop=stop),
                 reads=reads, writes=writes)

        def act(out, in_, func, reads, writes, bias=None, scale=None):
            kw = {}
            if bias is not None:
                kw["bias"] = bias
            if scale is not None:
                kw["scale"] = scale
            P.op("act", lambda e: e.activation(out=out, in_=in_, func=func, **kw), reads=reads, writes=writes)

        def tt(eng, out, in0, in1, op, reads, writes):
            P.op(eng, lambda e: e.tensor_tensor(out=out, in0=in0, in1=in1, op=op), reads=reads, writes=writes)

        def stt(eng, out, in0, scalar, in1, op0, op1, reads, writes):
            P.op(eng, lambda e: e.scalar_tensor_tensor(out=out, in0=in0, scalar=scalar, in1=in1, op0=op0, op1=op1),
                 reads=reads, writes=writes)

        def ts(eng, out, in0, s1, s2, op0, op1, reads, writes):
            if s2 is None:
                P.op(eng, lambda e: e.tensor_scalar(out=out, in0=in0, scalar1=s1, scalar2=None, op0=op0),
                     reads=reads, writes=writes)
            else:
                P.op(eng, lambda e: e.tensor_scalar(out=out, in0=in0, scalar1=s1, scalar2=s2, op0=op0, op1=op1),
                     reads=reads, writes=writes)

        def cp(eng, out, in_, reads, writes):
            if eng == "act":
                act(out, in_, AF.Copy, reads, writes)
            else:
                P.op(eng, lambda e: e.tensor_copy(out=out, in_=in_), reads=reads, writes=writes)

        def vcol(l, off, n=1):
            b = l * VL + off
            return vecs[:, b:b + n]

        G1, G2, BM, PSC, SUBL, LAM = 0, 8, 16, 64, 68, 69

        sTb = sb("sTb", [128, KC, NB + 1], BF16)

        def norm_mod(l, bcol, Aten, shift_which, xin, n, hout, rx, rh, hf_out=None):
            bi = bank()
            for k in range(KC):
                act(qsq(k, n), xin(k), AF.Square, [rx[k]], [R_sq[k % 2]])
                mm(pb[bi][:, 0:n], ones_bf[:, :], qsq(k, n), k == 0, k == KC - 1, [R_sq[k % 2], R_misc], [Rpb[bi]])
            rs, rrs = RS()
            act(rs[:, 0:n], pb[bi][:, 0:n], AF.Sqrt, [Rpb[bi], R_misc], [rrs], bias=epsc[:, 0:1], scale=1.0 / D)
            P.op("dve", lambda e: e.reciprocal(out=rs[:, 0:n], in_=rs[:, 0:n]), reads=[rrs], writes=[rrs])
            for k in range(KC):
                tq, rq = T()
                tt("dve", tq[:, 0:n], xin(k), rs[:, 0:n], ALU.mult, [rx[k], rrs], [rq])
                act(hout(k), tq[:, 0:n], AF.Identity, [rq, R_mod], [rh[k]],
                    bias=modT[:, l, shift_which * 8 + k, bcol:bcol + 1], scale=Aten[:, l, k, bcol:bcol + 1])
                if hf_out is not None:
                    hf_out(k, tq, rq)

        sqb = [sb(f"sqb{i}", [128, 512], BF16) for i in range(2)]
        rsb = [sb(f"rsb{i}", [128, 512], F32) for i in range(2)]
        R_rsb = [R("rsb0"), R("rsb1")]
        rs_ctr = [0]

        def RS():
            i = rs_ctr[0] % 2
            rs_ctr[0] += 1
            return rsb[i], R_rsb[i]

        R_sq = [R("sq0"), R("sq1")]
        epsc = sb("epsc", [128, 1], F32)

        def qsq(k, n):
            return sqb[k % 2][:, 0:n]

        def rope_store(src_bank, n, t0, dst, rdst, scale):
            i = bank_ctr[0] % 2
            q, rq = sqb[i], R_sq[i]
            act(q[:, 0:n], pb[src_bank][:, 0:n], AF.Copy, [Rpb[src_bank]], [rq], scale=scale)
            b2 = bank()
            mm(pb[b2][:, 0:n], rmat[:, :], q[:, 0:n], True, True, [rq, R_misc], [Rpb[b2]])
            t1, r1 = T()
            tt("dve", t1[:, 0:n], q[:, 0:n], cosT[:, t0:t0 + n], ALU.mult, [rq, R_const], [r1])
            t2, r2 = T()
            tt("dve", t2[:, 0:n], pb[b2][:, 0:n], sinT[:, t0:t0 + n], ALU.mult, [Rpb[b2], R_const], [r2])
            tt("pool", dst, t1[:, 0:n], t2[:, 0:n], ALU.add, [r1, r2], [rdst])

        def xs_tile(par, b, ti):
            return xs_d.ap()[par, b, ti]

        def key(b, ti):
            return (b, ti)

        sem_ld = [mksem(f"sem_ld{i}") for i in range(2)]
        sem_st = [mksem(f"sem_st{i}") for i in range(2)]

        def load_xT(par, b, ti, n):
            src = xs_tile(par, b, ti).rearrange("p (k t) -> p k t", k=KC)
            for k in range(KC):
                P.dma("sp", sem_ld[k % 2], xT[:, k, 0:n], src[:, k, 0:n], reads=[R_xs[(par, b, ti, k)]], writes=[R_xk[k]])

        def store_xT(par, b, ti, n):
            dst = xs_tile(par, b, ti).rearrange("p (k t) -> p k t", k=KC)
            for k in range(KC):
                r = R_xs.setdefault((par, b, ti, k), R(f"xs{par}_{b}_{ti}_{k}"))
                P.dma("sp", sem_st[k % 2], dst[:, k, 0:n], xT[:, k, 0:n], reads=[R_xk[k]], writes=[r])

        def phaseA0_tile(b, ti):
            n = C if ti == 0 else 512
            nblk = n // 128
            for blk in range(nblk):
                i = blk % 2
                if ti == 0:
                    src = ctx_d.ap()[b, blk * 128:(blk + 1) * 128, :]
                else:
                    t0 = (ti - 1) * 512 + blk * 128
                    src = x_d.ap()[b, t0:t0 + 128, :]
                P.dma("sp", sem_x[i], xtm[i], src, writes=R_hid[4 * i:4 * i + 4])
                for k in range(KC):
                    bi = bank()
                    mm(pb[bi][:, 0:128], xtm[i][:, k * 128:(k + 1) * 128], ident[:, :], True, True,
                       R_hid[4 * i:4 * i + 4] + [R_const], [Rpb[bi]])
                    cp("dve" if k % 2 else "act", xT[:, k, blk * 128:(blk + 1) * 128], pb[bi][:, 0:128], [Rpb[bi]], [R_xk[k]])
            store_xT(0, b, ti, n)
            if b == 0 and ti == 1:
                dump("xT0", xT[:, :, :], [128, KC, 512], F32, R_xk)

        def phaseA_tile(l, b, ti, from_sbuf):
            par = l % 2
            n = C if ti == 0 else 512
            kb = 0 if ti == 0 else C + (ti - 1) * 512
            bcol = NB if ti == 0 else b
            if not from_sbuf:
                load_xT(par, b, ti, n)
            norm_mod(l, bcol, A1, 0, lambda k: xT[:, k, 0:n], n, lambda k: hT[:, k, 0:n], R_xk, R_hk)
            if b == 0 and ti == 1:
                dump(f"hT_l{l}", hT[:, :, :], [128, KC, 512], BF16, R_hk)
            if b == 0 and ti == 0 and l == 0:
                dump(f"hcT_l{l}", hT[:, :, 0:C], [128, KC, C], BF16, R_hk)
            wap = w_in_d.ap()[l].rearrange("(k p) n -> p k n", p=128)
            for cb in range(2):
                wv, rw = wload(wap[:, :, OFF_K + cb * 512:OFF_K + (cb + 1) * 512], KC, 512, ("win", l, OFF_K + cb * 512))
                for hh_ in range(4):
                    h = cb * 4 + hh_
                    bi = bank()
                    for k in range(KC):
                        mm(pb[bi][:, 0:n], wv[:, k, hh_ * 128:(hh_ + 1) * 128], hT[:, k, 0:n], k == 0, k == KC - 1,
                           [rw, R_hk[k]], [Rpb[bi]])
                    rk = R_KT.setdefault((h, ti), R(f"KT{h}_{ti}"))
                    if ti == 0:
                        cp("act" if h % 2 else "dve", KT[:, h, kb:kb + n], pb[bi][:, 0:n], [Rpb[bi]], [rk])
                    else:
                        rope_store(bi, n, (ti - 1) * 512, KT[:, h, kb:kb + n], rk, 1.0)
            for cb in range(2):
                wv, rw = wload(wap[:, :, OFF_V + cb * 512:OFF_V + (cb + 1) * 512], KC, 512, ("win", l, OFF_V + cb * 512))
                for tb in range(n // 128):
                    bi = bank()
                    for k in range(KC):
                        mm(pb[bi][:, :], hT[:, k, tb * 128:(tb + 1) * 128], wv[:, k, :], k == 0, k == KC - 1,
                           [rw, R_hk[k]], [Rpb[bi]])
                    kc = kb // 128 + tb
                    rv = R_V.setdefault((kc, cb), R(f"V{kc}_{cb}"))
                    cp("act" if tb % 2 else "dve", Vt[:, kc, cb * 512:(cb + 1) * 512], pb[bi][:, :], [Rpb[bi]], [rv])

        def phaseB_tile(l, b, ti):
            par = l % 2
            last = (l == depth - 1)
            n = C if ti == 0 else 512
            bcol = NB if ti == 0 else b
            qpos = (ti - 1) * 512
            wap = w_in_d.ap()[l].rearrange("(k p) n -> p k n", p=128)
            load_xT(par, b, ti, n)
            has_l = ti >= 2
            has_r = (ti >= 1) and (ti < NT)
            if has_l:
                src = xs_tile(par, b, ti - 1).rearrange("p (k t) -> p k t", k=KC)[:, :, 512 - HAL:512]
                P.dma("sp", sem_xh, xh[:, :, 0:HAL], src, reads=[R_xs[(par, b, ti - 1, k)] for k in range(KC)], writes=[R_xh])
            if has_r:
                src = xs_tile(par, b, ti + 1).rearrange("p (k t) -> p k t", k=KC)[:, :, 0:HAL]
                P.dma("sp", sem_xh, xh[:, :, HAL:2 * HAL], src, reads=[R_xs[(par, b, ti + 1, k)] for k in range(KC)], writes=[R_xh])
            norm_mod(l, bcol, A1, 0, lambda k: xT[:, k, 0:n], n, lambda k: hT[:, k, 0:n], R_xk, R_hk)
            h0 = 0 if has_l else HAL
            h1 = 2 * HAL if has_r else HAL
            if has_l or has_r:
                if not has_l:
                    P.op("pool", lambda e: e.memset(xh[:, :, 0:HAL], 1.0), writes=[R_xh])
                if not has_r:
                    P.op("pool", lambda e: e.memset(xh[:, :, HAL:2 * HAL], 1.0), writes=[R_xh])
                norm_mod(l, bcol, A1, 0, lambda k: xh[:, k, :], 16, lambda k: hh[:, k, :], [R_xh] * KC, [R_hh] * KC)

            wv, rw = wload(wap[:, :, OFF_POOL:OFF_POOL + 512], KC, 512, ("win", l, OFF_POOL))
            for g in range(4):
                bi = bank()
                for k in range(KC):
                    mm(pb[bi][:, 0:n], wv[:, k, g * 128:(g + 1) * 128], hT[:, k, 0:n], k == 0, k == KC - 1,
                       [rw, R_hk[k]], [Rpb[bi]])
                cp("act", upad[:, g, HAL:HAL + n], pb[bi][:, 0:n], [Rpb[bi]], [R_up[g]])
                if has_l or has_r:
                    b2 = bank()
                    for k in range(KC):
                        mm(pb[b2][:, 0:16], wv[:, k, g * 128:(g + 1) * 128], hh[:, k, :], k == 0, k == KC - 1,
                           [rw, R_hh], [Rpb[b2]])
                    if has_l:
                        cp("dve", upad[:, g, 0:HAL], pb[b2][:, 0:HAL], [Rpb[b2]], [R_up[g]])
                    if has_r:
                        cp("dve", upad[:, g, HAL + n:HAL + n + HAL], pb[b2][:, HAL:2 * HAL], [Rpb[b2]], [R_up[g]])
                if not has_l:
                    P.op("pool", lambda e, g=g: e.memset(upad[:, g, 0:HAL], 0.0), writes=[R_up[g]])
                if not has_r:
                    P.op("pool", lambda e, g=g: e.memset(upad[:, g, HAL + n:HAL + n + HAL], 0.0), writes=[R_up[g]])
            W = n + 2 * HAL
            for g in range(4):
                cur, rcur = upad[:, g, :], R_up[g]
                lo, hi = 0, W
                sh = 0
                for step in range(g + 1):
                    tn, rn = T()
                    eng = "pool" if (g + step) % 2 else "dve"
                    if step == 0:
                        tt(eng, tn[:, 1:W], cur[:, 0:W - 1], cur[:, 1:W], ALU.add, [rcur], [rn])
                        lo, hi = 1, W
                    else:
                        d = 1 << (step - 1)
                        tt(eng, tn[:, lo + d:hi - d], cur[:, lo:hi - 2 * d], cur[:, lo + 2 * d:hi], ALU.add, [rcur], [rn])
                        lo, hi = lo + d, hi - d
                    cur, rcur = tn, rn
                assert lo <= HAL and hi >= HAL + n
                w = POOLW[g]
                stt("dve", AY[:, g, 0:n], cur[:, HAL:HAL + n], 1.0 / w, upad[:, g, HAL:HAL + n], ALU.mult, ALU.subtract,
                    [rcur, R_up[g]], [R_AY[g]])
                if not has_l:
                    te, re_ = T()
                    tt("dve", te[:, 0:HAL], cur[:, HAL:2 * HAL], edge[:, g, 0:HAL], ALU.mult, [rcur, R_const], [re_])
                    tt("dve", AY[:, g, 0:HAL], te[:, 0:HAL], upad[:, g, HAL:2 * HAL], ALU.subtract, [re_, R_up[g]], [R_AY[g]])
                if not has_r:
                    te, re_ = T()
                    tt("dve", te[:, 0:HAL], cur[:, n:n + HAL], edge[:, g, HAL:2 * HAL], ALU.mult, [rcur, R_const], [re_])
                    tt("dve", AY[:, g, n - HAL:n], te[:, 0:HAL], upad[:, g, n:n + HAL], ALU.subtract, [re_, R_up[g]], [R_AY[g]])
                bi = bank()
                mm(pb[bi][:, 0:n], poolw_l(l)[:, g, :], AY[:, g, 0:n], True, True, [R_AY[g], R_pw[l]], [Rpb[bi]])
                act(PY[:, g, 0:n], pb[bi][:, 0:n], AF.Copy, [Rpb[bi], R_const], [R_PY[g]], scale=vcol(l, PSC + g))

            if b == 0 and ti == 1:
                dump(f"PY_l{l}", PY[:, :, :], [128, 4, 512], BF16, R_PY)
            for cb in range(2):
                wv, rw = wload(wap[:, :, OFF_Q + cb * 512:OFF_Q + (cb + 1) * 512], KC, 512, ("win", l, OFF_Q + cb * 512))
                for hh_ in range(4):
                    h = cb * 4 + hh_
                    bi = bank()
                    for k in range(KC):
                        mm(pb[bi][:, 0:n], wv[:, k, hh_ * 128:(hh_ + 1) * 128], hT[:, k, 0:n], k == 0, k == KC - 1,
                           [rw, R_hk[k]], [Rpb[bi]])
                    if ti == 0:
                        act(QT[:, h, 0:n], pb[bi][:, 0:n], AF.Copy, [Rpb[bi]], [R_QT[h]], scale=0.125)
                    else:
                        rope_store(bi, n, qpos, QT[:, h, 0:n], R_QT[h], 0.125)

            if b == 0 and ti == 1:
                dump(f"QT_l{l}", QT[:, :, :], [128, NH, 512], BF16, R_QT)
            kcs = list(range(0, C // 128)) if ti == 0 else list(range(0, NKC))

            def kt_res(h, kc):
                t = 0 if kc < C // 128 else 1 + (kc * 128 - C) // 512
                return R_KT[(h, t)]

            for h in range(NH):
                def S_mm(j, kc):
                    for m in range(2):
                        bi = (j % 2) * 2 + m
                        mm(pb[bi][:, 0:n], KT[64 * m:64 * m + 64, h, kc * 128:(kc + 1) * 128],
                           QT[64 * m:64 * m + 64, h, 0:n], True, True, [kt_res(h, kc), R_QT[h]], [Rpb[bi]])
                S_mm(0, kcs[0])
                for j, kc in enumerate(kcs):
                    if j + 1 < len(kcs):
                        S_mm(j + 1, kcs[j + 1])
                    for m in range(2):
                        bi = (j % 2) * 2 + m
                        act(PT[bi][:, 0:n], pb[bi][:, 0:n], AF.Exp, [Rpb[bi]], [R_PT[bi]])
                    for m in range(2):
                        bi = (j % 2) * 2 + m
                        mm(pb[4 + m][:, 0:n], Vt[:, kc, h * 128:(h + 1) * 128], PT[bi][:, 0:n], j == 0, j == len(kcs) - 1,
                           [R_V[(kc, h // 4)], R_PT[bi]], [Rpb[4 + m]])
                        mm(pb[6 + m][:, 0:n], ones_bf[:, :], PT[bi][:, 0:n], j == 0, j == len(kcs) - 1,
                           [R_misc, R_PT[bi]], [Rpb[6 + m]])
                r1, rr1 = T()
                P.op("dve", lambda e, r1=r1: e.reciprocal(out=r1[:, 0:n], in_=pb[6][:, 0:n]), reads=[Rpb[6]], writes=[rr1])
                r2, rr2 = T()
                P.op("dve", lambda e, r2=r2: e.reciprocal(out=r2[:, 0:n], in_=pb[7][:, 0:n]), reads=[Rpb[7]], writes=[rr2])
                o1, ro1 = T()
                tt("dve", o1[:, 0:n], pb[4][:, 0:n], r1[:, 0:n], ALU.mult, [Rpb[4], rr1], [ro1])
                tt("dve", r2[:, 0:n], pb[5][:, 0:n], r2[:, 0:n], ALU.mult, [Rpb[5], rr2], [rr2])
                stt("dve", AY[:, h, 0:n], r2[:, 0:n], lamv[:, l, 0:1], o1[:, 0:n], ALU.mult, ALU.add, [rr2, ro1, R_mod], [R_AY[h]])
            for h in range(NH):
                i = h % 2
                act(sqb[i][:, 0:n], AY[:, h, 0:n], AF.Square, [R_AY[h]], [R_sq[i]])
                bi = bank()
                mm(pb[bi][:, 0:n], ones_bf[:, :], sqb[i][:, 0:n], True, True, [R_sq[i], R_misc], [Rpb[bi]])
                r1, rr1 = T()
                act(r1[:, 0:n], pb[bi][:, 0:n], AF.Sqrt, [Rpb[bi], R_misc], [rr1], bias=epsc[:, 0:1], scale=1.0 / 128)
                P.op("dve", lambda e, r1=r1: e.reciprocal(out=r1[:, 0:n], in_=r1[:, 0:n]), reads=[rr1], writes=[rr1])
                stt("dve", AY[:, h, 0:n], AY[:, h, 0:n], lamv[:, l, 1:2], r1[:, 0:n], ALU.mult, ALU.mult, [R_AY[h], rr1, R_mod], [R_AY[h]])

            if b == 0 and ti == 1:
                dump(f"AY_l{l}", AY[:, :, :], [128, NH, 512], BF16, R_AY)
            for half in range(2):
                for gsel_ in range(2):
                    wg, rwg = wload(wap[:, :, OFF_G + gsel_ * D + half * 512:OFF_G + gsel_ * D + (half + 1) * 512], KC, 512, ("win", l, OFF_G + gsel_ * D + half * 512))
                    for cc in range(4):
                        bi = bank()
                        for k in range(KC):
                            mm(pb[bi][:, 0:n], wg[:, k, cc * 128:(cc + 1) * 128], hT[:, k, 0:n], k == 0, k == KC - 1,
                               [rwg, R_hk[k]], [Rpb[bi]])
                        act(hid[:, gsel_ * 4 + cc, 0:n], pb[bi][:, 0:n], AF.Sigmoid, [Rpb[bi]], [R_hid[gsel_ * 4 + cc]])
                wpp, rwpp = wload(w_pp_d.ap()[l].rearrange("(k p) n -> p k n", p=128)[:, :, half * 512:(half + 1) * 512], 4, 512, ("wpp", l, half))
                wat, rwat = wload(w_ap_d.ap()[l].rearrange("(k p) n -> p k n", p=128)[:, :, half * 512:(half + 1) * 512], KC, 512, ("wap", l, half))
                for cc in range(4):
                    c = half * 4 + cc
                    b_pp, b_ap = bank(), bank()
                    for k in range(4):
                        mm(pb[b_pp][:, 0:n], wpp[:, k, cc * 128:(cc + 1) * 128], PY[:, k, 0:n], k == 0, k == 3,
                           [rwpp, R_PY[k]], [Rpb[b_pp]])
                    for k in range(KC):
                        mm(pb[b_ap][:, 0:n], wat[:, k, cc * 128:(cc + 1) * 128], AY[:, k, 0:n], k == 0, k == KC - 1,
                           [rwat, R_AY[k]], [Rpb[b_ap]])
                    t1, rt1 = T()
                    tt("dve", t1[:, 0:n], pb[b_pp][:, 0:n], hid[:, cc, 0:n], ALU.mult, [Rpb[b_pp], R_hid[cc]], [rt1])
                    t2, rt2 = T()
                    tt("dve", t2[:, 0:n], pb[b_ap][:, 0:n], hid[:, 4 + cc, 0:n], ALU.mult, [Rpb[b_ap], R_hid[4 + cc]], [rt2])
                    tt("pool", QT[:, c, 0:n], t1[:, 0:n], t2[:, 0:n], ALU.add, [rt1, rt2], [R_QT[c]])
            if b == 0 and ti == 1:
                dump(f"YM_l{l}", QT[:, :, :], [128, NH, 512], BF16, R_QT)
            for half in range(2):
                wv, rw = wload(w_out_d.ap()[l].rearrange("(k p) n -> p k n", p=128)[:, :, half * 512:(half + 1) * 512], KC, 512, ("wout", l, half))
                for cc in range(4):
                    c = half * 4 + cc
                    bi = bank()
                    for k in range(KC):
                        mm(pb[bi][:, 0:n], wv[:, k, cc * 128:(cc + 1) * 128], QT[:, k, 0:n], k == 0, k == KC - 1,
                           [rw, R_QT[k]], [Rpb[bi]])
                    stt("dve", xT[:, c, 0:n], pb[bi][:, 0:n], modT[:, l, 16 + c, bcol:bcol + 1], xT[:, c, 0:n], ALU.mult, ALU.add,
                        [Rpb[bi], R_mod, R_xk[c]], [R_xk[c]])

            if b == 0 and ti == 1:
                dump(f"xmix_l{l}", xT[:, :, :], [128, KC, 512], F32, R_xk)
            if l % 2 == 0:
                norm_mod(l, bcol, A2, 3, lambda k: xT[:, k, 0:n], n, lambda k: hT[:, k, 0:n], R_xk, R_hk)
                i_l = l // 2
                w1ap = ffn_w1_d.ap()[i_l].rearrange("(k p) n -> p k n", p=128)
                w3ap = ffn_w3_d.ap()[i_l].rearrange("(k p) n -> p k n", p=128)
                ffn_half(l, bcol, n, w1ap, w3ap, ffn_w2_d.ap()[i_l], 0, 11 * 128, None, ("ffn", i_l))
                ffn_half(l, bcol, n, w1ap, w3ap, ffn_w2_d.ap()[i_l], 11 * 128, DFF, None, ("ffn", i_l))
            else:
                i_l = l // 2
                moe(l, i_l, bcol, n)

            if b == 0 and ti == 1:
                dump(f"xffn_l{l}", xT[:, :, :], [128, KC, 512], F32, R_xk)
            if last:
                if ti >= 1:
                    final_out(b, ti, n)
            else:
                store_xT(1 - par, b, ti, n)

        def ffn_half(l, bcol, n, w1ap, w3ap, w2ap, f0, f1, gate_bc, wkey):
            nf = f1 - f0
            nch = (nf + 127) // 128
            ci = 0
            for c0 in range(0, nf, 512):
                cw = min(512, nf - c0)
                w1v, rw1 = wload(w1ap[:, :, f0 + c0:f0 + c0 + cw], KC, cw, ("w1", wkey, f0 + c0))
                w3v, rw3 = wload(w3ap[:, :, f0 + c0:f0 + c0 + cw], KC, cw, ("w3", wkey, f0 + c0))
                for cc in range((cw + 127) // 128):
                    m = min(128, cw - cc * 128)
                    b1, b3 = bank(), bank()
                    for k in range(KC):
                        mm(pb[b1][0:m, 0:n], w1v[:, k, cc * 128:cc * 128 + m], hT[:, k, 0:n], k == 0, k == KC - 1,
                           [rw1, R_hk[k]], [Rpb[b1]])
                    for k in range(KC):
                        mm(pb[b3][0:m, 0:n], w3v[:, k, cc * 128:cc * 128 + m], hT[:, k, 0:n], k == 0, k == KC - 1,
                           [rw3, R_hk[k]], [Rpb[b3]])
                    gi = 2 + ci % 2
                    act(gbf[gi][0:m, 0:n], pb[b1][0:m, 0:n], AF.Silu, [Rpb[b1]], [R_gbf[gi]])
                    if gate_bc is None:
                        tt("dve", hid[0:m, ci, 0:n], pb[b3][0:m, 0:n], gbf[gi][0:m, 0:n], ALU.mult, [Rpb[b3], R_gbf[gi]], [R_hid[ci]])
                    else:
                        t1, rt1 = T()
                        tt("dve", t1[0:m, 0:n], pb[b3][0:m, 0:n], gbf[gi][0:m, 0:n], ALU.mult,
                           [Rpb[b3], R_gbf[gi]], [rt1])
                        tt("pool", hid[0:m, ci, 0:n], t1[0:m, 0:n], gate_bc[0][0:m, 0:n], ALU.mult,
                           [rt1, gate_bc[1]], [R_hid[ci]])
                    ci += 1
            assert ci == nch
            nfull = nf // 128
            rem = nf - nfull * 128
            for cg in range(4):
                s_ = w_ctr[0] % cfg.NW
                w_ctr[0] += 1
                flat = wsl[s_][:, 0:nch * 256]
                view = flat.rearrange("p (k c) -> p k c", k=nch)
                key = ("w2", wkey, f0, cg)
                if key in wcache:
                    scr, rscr = wcache[key]
                    P.dma("sp", wsem[s_], flat, scr.ap(), reads=[rscr], writes=[Rw[s_]])
                else:
                    if nfull > 0:
                        P.dma("pool", wsem[s_], view[:, 0:nfull, :],
                              w2ap[f0:f0 + nfull * 128, cg * 256:(cg + 1) * 256].rearrange("(k p) n -> p k n", p=128),
                              writes=[Rw[s_]])
                    if rem:
                        P.dma("pool", wsem[s_], view[0:rem, nfull, :], w2ap[f0 + nfull * 128:f1, cg * 256:(cg + 1) * 256],
                              writes=[Rw[s_]])
                    if getattr(cfg, "wcache", True):
                        scr = nc.dram_tensor(f"ws{len(wcache)}", [128, nch * 256], BF16)
                        rscr = R(f"ws{len(wcache)}")
                        P.dma("sp", ssem[s_], scr.ap(), flat, reads=[Rw[s_]], writes=[rscr])
                        wcache[key] = (scr, rscr)
                for cc in range(2):
                    c = cg * 2 + cc
                    bi = bank()
                    for j in range(nch):
                        m = 128 if j < nfull else rem
                        mm(pb[bi][:, 0:n], view[0:m, j, cc * 128:(cc + 1) * 128], hid[0:m, j, 0:n], j == 0, j == nch - 1,
                           [Rw[s_], R_hid[j]], [Rpb[bi]])
                    stt("dve", xT[:, c, 0:n], pb[bi][:, 0:n], modT[:, l, 40 + c, bcol:bcol + 1], xT[:, c, 0:n], ALU.mult, ALU.add,
                        [Rpb[bi], R_mod, R_xk[c]], [R_xk[c]])

        moe_dumped = []

        def moe(l, i_l, bcol, n):
            nblk = n // 128
            lgb = bank()

            def hf_out(k, tq, rq):
                hf, rhf = T()
                act(hf[:, 0:n], tq[:, 0:n], AF.Identity, [rq, R_mod], [rhf],
                    bias=modT[:, l, 3 * 8 + k, bcol:bcol + 1], scale=A2[:, l, k, bcol:bcol + 1])
                i = k % 4
                tt("dve", PT[i][:, 0:n], hf[:, 0:n], hT[:, k, 0:n], ALU.subtract, [rhf, R_hk[k]], [R_PT[i]])
                mm(pb[lgb][0:8, 0:n], wrh[:, k, :], hT[:, k, 0:n], k == 0, False, [R_misc, R_hk[k]], [Rpb[lgb]])
                mm(pb[lgb][0:8, 0:n], wrh[:, k, :], PT[i][:, 0:n], False, False, [R_misc, R_PT[i]], [Rpb[lgb]])
                mm(pb[lgb][0:8, 0:n], wrl[:, k, :], hT[:, k, 0:n], False, k == KC - 1, [R_misc, R_hk[k]], [Rpb[lgb]])
            norm_mod(l, bcol, A2, 3, lambda k: xT[:, k, 0:n], n, lambda k: hT[:, k, 0:n], R_xk, R_hk, hf_out=hf_out)
            lgT, rlgT = T()
            cp("dve", lgT[0:8, 0:n], pb[lgb][0:8, 0:n], [Rpb[lgb]], [rlgT])
            cp("dve", gsel[0][:, 0:n], lgT[0:8, 0:n], [rlgT], [R_gs[0]])
            tt("dve", gsel[1][:, 0:n], lgT[0:8, 0:n], gsel[0][:, 0:n], ALU.subtract, [rlgT, R_gs[0]], [R_gs[1]])
            lb2 = bank()
            for tb in range(nblk):
                mm(pb[lb2][:, tb * 8:tb * 8 + 8], gsel[0][0:8, tb * 128:(tb + 1) * 128], identb[0:8, 0:8], True, False,
                   [R_gs[0], R_misc], [Rpb[lb2]])
                mm(pb[lb2][:, tb * 8:tb * 8 + 8], gsel[1][0:8, tb * 128:(tb + 1) * 128], identb[0:8, 0:8], False, True,
                   [R_gs[1], R_misc], [Rpb[lb2]])
            cp("dve", lg[:, 0:nblk, :], pb[lb2][:, 0:nblk * 8].rearrange("p (a e) -> p a e", e=8), [Rpb[lb2]], [R_lg])
            for tb in range(nblk):
                P.op("dve", lambda e, tb=tb: e.max(out=mx8[:, tb, :], in_=lg[:, tb, :]), reads=[R_lg], writes=[R_gate])
            tt("dve", sm[:, 0:nblk], mx8[:, 0:nblk, 1], mx8[:, 0:nblk, 0], ALU.subtract, [R_gate], [R_gate])
            act(sm[:, 4:4 + nblk], sm[:, 0:nblk], AF.Exp, [R_gate], [R_gate])
            ts("dve", sm[:, 4:4 + nblk], sm[:, 4:4 + nblk], 1.0, None, ALU.add, None, [R_gate], [R_gate])
            P.op("dve", lambda e: e.reciprocal(out=sm[:, 8:8 + nblk], in_=sm[:, 4:4 + nblk]), reads=[R_gate], writes=[R_gate])
            ts("dve", sm[:, 12:12 + nblk], sm[:, 8:8 + nblk], -1.0, 1.0, ALU.mult, ALU.add, [R_gate], [R_gate])
            for tb in range(nblk):
                ts("dve", gt[:, tb, :], lg[:, tb, :], mx8[:, tb, 0:1], sm[:, 8 + tb:9 + tb], ALU.is_equal, ALU.mult, [R_lg, R_gate], [R_gate])
                t1, rt1 = T()
                ts("dve", t1[:, 0:8], lg[:, tb, :], mx8[:, tb, 1:2], sm[:, 12 + tb:13 + tb], ALU.is_equal, ALU.mult, [R_lg, R_gate], [rt1])
                tt("dve", gtb[:, tb, :], gt[:, tb, :], t1[:, 0:8], ALU.add, [R_gate, rt1], [R_gate])
            gb = bank()
            for tb in range(nblk):
                mm(pb[gb][0:8, tb * 128:(tb + 1) * 128], gtb[:, tb, :], identb[:, :], True, True, [R_gate, R_misc], [Rpb[gb]])
            cp("act", gT8[:, 0:n], pb[gb][0:8, 0:n], [Rpb[gb]], [R_gate])
            if bcol == 0 and n == 512 and not moe_dumped:
                moe_dumped.append(1)
                dump("lg", lg[:, :, :], [128, 4, NE], F32, [R_lg])
                dump("mx8", mx8[:, :, :], [128, 4, 8], F32, [R_gate])
                dump("gt", gtb[:, :, :], [128, 4, NE], BF16, [R_gate])
                dump("gT8", gT8[:, :], [8, 512], BF16, [R_gate])
            for e in range(NE):
                gb = bank()
                gi = e % 2
                ts("dve", gsel[gi][:, 0:n], gT8[:, 0:n], identf8(e), None, ALU.mult, None, [R_gate, R_const], [R_gs[gi]])
                mm(pb[gb][:, 0:n], ones_bf[0:8, :], gsel[gi][:, 0:n], True, True, [R_gs[gi], R_misc], [Rpb[gb]])
                cp("act", gbf[gi][:, 0:n], pb[gb][:, 0:n], [Rpb[gb]], [R_gbf[gi]])
                w1ap = moe_w1_d.ap()[i_l, e].rearrange("(k p) n -> p k n", p=128)
                w3ap = moe_w3_d.ap()[i_l, e].rearrange("(k p) n -> p k n", p=128)
                ffn_half(l, bcol, n, w1ap, w3ap, moe_w2_d.ap()[i_l, e], 0, DFE, (gbf[gi], R_gbf[gi]), ("moe", i_l, e))

        identb = sb("identb", [128, 128], BF16)
        R_gs = [R("gs0"), R("gs1")]

        def identf8(e):
            return ident[0:8, e:e + 1]

        def final_out(b, ti, n):
            bi = bank()
            for k in range(KC):
                act(qsq(k, n), xT[:, k, 0:n], AF.Square, [R_xk[k]], [R_sq[k % 2]])
                mm(pb[bi][:, 0:n], ones_bf[:, :], qsq(k, n), k == 0, k == KC - 1, [R_sq[k % 2], R_misc], [Rpb[bi]])
            rs, rrs = RS()
            act(rs[:, 0:n], pb[bi][:, 0:n], AF.Sqrt, [Rpb[bi], R_misc], [rrs], bias=epsc[:, 0:1], scale=1.0 / D)
            P.op("dve", lambda e: e.reciprocal(out=rs[:, 0:n], in_=rs[:, 0:n]), reads=[rrs], writes=[rrs])
            for k in range(KC):
                stt("dve", xT[:, k, 0:n], xT[:, k, 0:n], vecs[:, 2 * VL + k:2 * VL + k + 1], rs[:, 0:n], ALU.mult, ALU.mult,
                    [R_xk[k], rrs, R_const], [R_xk[k]])
            for blk in range(n // 128):
                i = blk % 2
                for half in range(2):
                    bi = bank()
                    for kk in range(4):
                        k = half * 4 + kk
                        mm(pb[bi][:, kk * 128:(kk + 1) * 128], xT[:, k, blk * 128:(blk + 1) * 128], ident[:, :], True, True,
                           [R_xk[k], R_const], [Rpb[bi]])
                    cp("act" if half else "dve", xtm[i][:, half * 512:(half + 1) * 512], pb[bi][:, :], [Rpb[bi]], R_hid[4 * i:4 * i + 4])
                t0 = (ti - 1) * 512 + blk * 128
                o = P.dma("sp", sem_o[i], out_d.ap()[b, t0:t0 + 128, :], xtm[i], reads=R_hid[4 * i:4 * i + 4])
                fw_last[i] = (sem_o[i], o.dval)

        fw_last = {}
        R_pw = [R("pw0"), R("pw1")]
        poolw2 = sb("poolw2", [128, 2, 4, 128], BF16)

        def poolw_l(l):
            return poolw2[:, l]

        P.op("dve", lambda e: e.memset(epsc[:, :], EPS), writes=[R_misc])
        def run_prologue():
            P.dma("sp", sem_c, ident[:, :], ident_d.ap()[:, :], writes=[R_const])
            P.dma("sp", sem_c, vecs[:, :], vecs_d.ap()[:, :], writes=[R_const])
            P.dma("pool", sem_sp, cosT[:, :], cos_d.ap()[:, :], writes=[R_const])
            P.dma("pool", sem_sp, sinT[:, :], sin_d.ap()[:, :], writes=[R_const])
            P.dma("sp", sem_c, edge[:, :, :], edge_d.ap()[:, :, :], writes=[R_const])
            P.dma("sp", sem_c, sT[:, :, :], cT_d.ap()[:, :, :], writes=[R_const])
            P.dma("sp", sem_c, wr[:, :, :], router_d.ap()[0].rearrange("(k p) e -> p k e", p=128), writes=[R_const])
            P.dma("pool", sem_sp, rmat[:, :], rmat_d.ap()[:, :], writes=[R_misc])
            P.dma("pool", sem_sp, identb[:, :], ident_d.ap()[:, :], writes=[R_misc])
            for l in range(depth):
                P.dma("pool", sem_sp, poolw2[:, l], pool_w_d.ap()[l].rearrange("g c d -> c g d"), writes=[R_pw[l]])
            P.op("dve", lambda e: e.memset(ones_bf[:, :], 1.0), writes=[R_misc])
            P.op("dve", lambda e: e.memset(ones_f[:, :], 1.0), writes=[R_misc])
            act(sT[:, :, :], sT[:, :, :], AF.Silu, [R_const], [R_const])
            cp("dve", sTb[:, :, :], sT[:, :, :], [R_const], [R_misc])
            cp("dve", wrh[:, :, :], wr[:, :, :], [R_const], [R_misc])
            tt("dve", wrl[:, :, :], wr[:, :, :], wrh[:, :, :], ALU.subtract, [R_const, R_misc], [R_misc])
            nb1 = NB + 1
            for l in range(depth):
                for jb in range(12):
                    wv, rw = wload(w_mod_d.ap()[l].rearrange("(k p) n -> p k n", p=128)[:, :, jb * 512:(jb + 1) * 512], KC, 512)
                    bi = bank()
                    for jj in range(4):
                        for k in range(KC):
                            mm(pb[bi][:, jj * 8:jj * 8 + nb1], wv[:, k, jj * 128:(jj + 1) * 128], sTb[:, k, :],
                               k == 0, k == KC - 1, [rw, R_misc], [Rpb[bi]])
                    for jj in range(4):
                        j = jb * 4 + jj
                        act(modT[:, l, j, :], pb[bi][:, jj * 8:jj * 8 + nb1], AF.Identity, [Rpb[bi], R_const], [R_mod],
                            bias=vcol(l, BM + j), scale=1.0)
                for k in range(KC):
                    ts("dve", A1[:, l, k, :], modT[:, l, 8 + k, :], 1.0, vcol(l, G1 + k), ALU.add, ALU.mult, [R_mod, R_const], [R_mod])
                    ts("dve", A2[:, l, k, :], modT[:, l, 32 + k, :], 1.0, vcol(l, G2 + k), ALU.add, ALU.mult, [R_mod, R_const], [R_mod])
                t0, r0 = T()
                tt("dve", t0[0:64, 0:1], vcol(l, LAM + 0)[0:64, :], vcol(l, LAM + 1)[0:64, :], ALU.mult, [R_const], [r0])
                tt("dve", t0[0:64, 1:2], vcol(l, LAM + 2)[0:64, :], vcol(l, LAM + 3)[0:64, :], ALU.mult, [R_const], [r0])
                bi = bank()
                cp("dve", sqb[0][0:64, 0:2], t0[0:64, 0:2], [r0], [R_sq[0]])
                tt("dve", sqb[0][0:64, 2:4], t0[0:64, 0:2], sqb[0][0:64, 0:2], ALU.subtract, [r0, R_sq[0]], [R_sq[0]])
                mm(pb[bi][:, 0:2], ones_bf[0:64, :], sqb[0][0:64, 0:2], True, False, [R_sq[0], R_misc], [Rpb[bi]])
                mm(pb[bi][:, 0:2], ones_bf[0:64, :], sqb[0][0:64, 2:4], False, True, [R_sq[0], R_misc], [Rpb[bi]])
                t1, r1 = T()
                act(t1[:, 0:2], pb[bi][:, 0:2], AF.Exp, [Rpb[bi]], [r1])
                lam0 = lambda_init(l)
                tt("dve", t1[:, 2:3], t1[:, 1:2], t1[:, 0:1], ALU.subtract, [r1], [r1])
                ts("dve", lamv[:, l, 0:1], t1[:, 2:3], -lam0, None, ALU.add, None, [r1], [R_mod])
                ts("dve", lamv[:, l, 1:2], vcol(l, SUBL), 1.0 - lam0, None, ALU.mult, None, [R_const], [R_mod])

        cfg.sbuf_left = nc.sbuf_bytes_remaining
        run_prologue()
        dump("modT", modT[:, :, :, :], [128, 2, 48, NB + 1], F32, [R_mod])
        dump("lamv", lamv[:, :, :], [128, 2, 4], F32, [R_mod])
        dump("A1", A1[:, :, :, :], [128, 2, KC, NB + 1], F32, [R_mod])

        for b in range(NB):
            for ti in range(NT + 1):
                phaseA0_tile(b, ti)
                phaseA_tile(0, b, ti, from_sbuf=True)
            for l in range(depth):
                last = (l == depth - 1)
                if l > 0:
                    for ti in range(NT + 1):
                        phaseA_tile(l, b, ti, from_sbuf=False)
                tiles = list(range(1, NT + 1)) if last else list(range(0, NT + 1))
                if b == 0:
                    dump(f"KT_l{l}", KT[:, :, :], [128, NH, S], BF16, list(R_KT.values()))
                    dump(f"V_l{l}", Vt[:, :, :], [128, NKC, D], BF16, list(R_V.values()))
                for ti in tiles:
                    phaseB_tile(l, b, ti)

        for i in fw_last:
            final_waits.append(fw_last[i])
        P.emit(mksem, final_waits)
    return nc, P


def host_consts(L):
    ident = np.eye(128, dtype=np.float32)
    p = np.arange(128)
    j = p % 64
    axis = j // 32
    ab = (j % 32) // 16
    pair = j % 16
    partner = np.where(ab == 0, p + 16, p - 16)
    rmat = np.zeros((128, 128), np.float32)
    rmat[partner, p] = 1.0
    inv = (10000.0 ** (-np.arange(16, dtype=np.float32) * 2.0 / 32)).astype(np.float32)
    t = np.arange(L)
    pos = np.stack([(t // GRID_W).astype(np.float32), (t % GRID_W).astype(np.float32)], 0)
    ang = pos[axis] * inv[pair][:, None]
    cosT = np.cos(ang).astype(np.float32)
    sgn = np.where(ab == 0, -1.0, 1.0).astype(np.float32)[:, None]
    sinT = (np.sin(ang).astype(np.float32) * sgn).astype(np.float32)
    edge = np.zeros((128, 4, 16), np.float32)
    BIG = 1 << 20
    for g, w in enumerate(POOLW):
        for tt_ in range(8):
            lo = max(tt_ - w // 2, 0)
            hi = tt_ + w // 2
            edge[:, g, tt_] = 1.0 / (hi - lo)
            tpos = BIG - 8 + tt_
            lo = tpos - w // 2
            hi = min(tpos + w // 2, BIG)
            edge[:, g, 8 + tt_] = 1.0 / (hi - lo)
    sel = np.zeros((8, 8 * 128), np.float32)
    for e in range(8):
        sel[e, e * 128:(e + 1) * 128] = 1.0
    return ident, rmat, cosT, sinT, edge, sel


def pack_vecs(inp):
    v = np.zeros((128, NV), np.float32)
    for l in range(2):
        b = l * VL
        v[:, b + 0:b + 8] = inp["norm1_g"][l].reshape(8, 128).T
        v[:, b + 8:b + 16] = inp["norm2_g"][l].reshape(8, 128).T
        v[:, b + 16:b + 64] = inp["b_mod"][l].reshape(48, 128).T
        v[:, b + 64:b + 68] = inp["pool_scale"][l].reshape(4, 128).T
        v[:, b + 68] = inp["subln_g"][l]
        v[0:64, b + 69] = inp["lam_q1"][l]
        v[0:64, b + 70] = inp["lam_k1"][l]
        v[0:64, b + 71] = inp["lam_q2"][l]
        v[0:64, b + 72] = inp["lam_k2"][l]
    v[:, 2 * VL:2 * VL + 8] = inp["final_g"].reshape(8, 128).T
    return v


_CACHE = {}


def run(inp, cfg, n_cores):
    keyc = (cfg.NB, cfg.L, cfg.C, cfg.depth, cfg.NW)
    if keyc not in _CACHE:
        _CACHE[keyc] = build(cfg)
    nc, P = _CACHE[keyc]
    ident, rmat, cosT, sinT, edge, sel = host_consts(cfg.L)
    vecs = pack_vecs(inp)
    f = lambda a: np.ascontiguousarray(np.asarray(a, dtype=np.float32))
    shared = {
        "vecs": vecs, "ident": ident, "rmat": rmat, "cosT": cosT, "sinT": sinT, "edge": edge, "sel": sel,
        "w_mod": f(inp["w_mod"]), "w_in": f(inp["w_in"]), "pool_w": f(inp["pool_w"]),
        "w_pool_proj": f(inp["w_pool_proj"]), "w_attn_proj": f(inp["w_attn_proj"]), "w_out": f(inp["w_out"]),
        "ffn_w1": f(inp["ffn_w1"]), "ffn_w3": f(inp["ffn_w3"]), "ffn_w2": f(inp["ffn_w2"]),
        "router_w": f(inp["router_w"]), "moe_w1": f(inp["moe_w1"]), "moe_w3": f(inp["moe_w3"]), "moe_w2": f(inp["moe_w2"]),
    }
    NB = cfg.NB
    in_maps = []
    x = f(inp["x"])
    ctx = f(inp["ctx"])
    c = f(inp["c"])
    c_ctx = f(inp["c_ctx"])
    for i in range(n_cores):
        cc = np.concatenate([c[i * NB:(i + 1) * NB], c_ctx[None, :]], 0)
        cT = np.ascontiguousarray(cc.reshape(NB + 1, 8, 128).transpose(2, 1, 0))
        m = dict(shared)
        m["x"] = np.ascontiguousarray(x[i * NB:(i + 1) * NB])
        m["ctx"] = np.ascontiguousarray(ctx[i * NB:(i + 1) * NB])
        m["cT"] = cT
        in_maps.append(m)
    res = run_bass_kernel_spmd(nc, in_maps, core_ids=list(range(n_cores)))
    if getattr(cfg, "debug", False):
        cfg.dbg = {k: np.asarray(res.results[0][k]) for k in cfg.dbg_names}
    return np.concatenate([r["out"] for r in res.results], axis=0)


def kernel(**inputs):
    cfg = Cfg(NB=4, L=2048, C=256, depth=2, NW=3)
    return run(inputs, cfg, 8)
```
